# Optimizing a Trainium2 kernel written in Bass

```python
import math
import jax, jax.numpy as jnp
from jax import lax
import numpy as np

D_MODEL = 1024
BATCH = 16
SEQ = 2048
DEPTH = 4

SB_HEADS = 4
SB_HD = 128
SB_W = SB_HEADS * SB_HD
SB_BLOCK = 128
GDN_HEADS = 4
GDN_HD = 128
GDN_W = GDN_HEADS * GDN_HD
GDN_CONV = 4
GDN_CHUNK = 64
GLA_HEADS = 4
GLA_KD = 128
GLA_VD = 256
GLA_KW = GLA_HEADS * GLA_KD
GLA_VW = GLA_HEADS * GLA_VD
GLA_RANK = 16
GLA_TAU = 16.0
GLA_CHUNK = 64
N_BRANCH = 3
RMS_EPS = 1e-6
L2_EPS = 1e-6

IN_SPLITS = (SB_W, SB_W, SB_W, SB_W,
             GDN_W, GDN_W, GDN_W, GDN_W, GDN_HEADS, GDN_HEADS,
             GLA_KW, GLA_KW, GLA_VW, GLA_VW, GLA_RANK,
             N_BRANCH * D_MODEL)
N_IN = sum(IN_SPLITS)

kernel_name = "hybrid_sb_gdn_gla_adaln"


def rms_norm(x, g):
    xf = x.astype(jnp.float32)
    y = xf * lax.rsqrt(jnp.mean(xf * xf, axis=-1, keepdims=True) + RMS_EPS)
    return (y * g.astype(jnp.float32)).astype(x.dtype)


def l2_norm(x):
    xf = x.astype(jnp.float32)
    return xf * lax.rsqrt(jnp.sum(xf * xf, axis=-1, keepdims=True) + L2_EPS)


def split_heads(t, n_heads):
    b, s, w = t.shape
    return t.reshape(b, s, n_heads, w // n_heads).transpose(0, 2, 1, 3)


def merge_heads(t):
    b, h, s, d = t.shape
    return t.transpose(0, 2, 1, 3).reshape(b, s, h * d)


def causal_depthwise_conv(x, w):
    kw = w.shape[0]
    return lax.conv_general_dilated(
        x, w[:, None, :].astype(x.dtype), window_strides=(1,), padding=[(kw - 1, 0)],
        dimension_numbers=('NWC', 'WIO', 'NWC'), feature_group_count=x.shape[-1])


def stick_breaking_attention(q, k, v):
    b, h, s, d = q.shape
    nb = s // SB_BLOCK
    scale = d ** -0.5
    key_pos = jnp.arange(s)
    qb = q.reshape(b, h, nb, SB_BLOCK, d).transpose(2, 0, 1, 3, 4)

    def block(args):
        q_blk, i = args
        z = jnp.einsum('bhqd,bhkd->bhqk', q_blk, k) * scale
        q_pos = i * SB_BLOCK + jnp.arange(SB_BLOCK)
        mask = key_pos[None, :] < q_pos[:, None]
        log_1m = jnp.where(mask, jax.nn.log_sigmoid(-z), 0.0)
        tail = lax.cumsum(log_1m, axis=3, reverse=True) - log_1m
        w = jnp.where(mask, jnp.exp(jax.nn.log_sigmoid(z) + tail), 0.0)
        return jnp.einsum('bhqk,bhkd->bhqd', w, v)

    out = lax.map(block, (qb, jnp.arange(nb)))
    return out.transpose(1, 2, 0, 3, 4).reshape(b, h, s, d)


def gated_delta_rule(q, k, v, beta, g):
    b, h, s, dk = q.shape
    dv = v.shape[-1]
    c = GDN_CHUNK
    n = s // c
    q, k, v = (t.reshape(b, h, n, c, t.shape[-1]) for t in (q, k, v))
    beta = beta.reshape(b, h, n, c)
    gam = jnp.cumsum(g.reshape(b, h, n, c), axis=-1)
    tri_incl = jnp.tril(jnp.ones((c, c), bool))
    tri_strict = jnp.tril(jnp.ones((c, c), bool), -1)
    diff = gam[..., :, None] - gam[..., None, :]
    decay = jnp.where(tri_incl, jnp.exp(jnp.where(tri_incl, diff, 0.0)), 0.0)
    a_mat = jnp.where(tri_strict,
                      beta[..., None] * jnp.einsum('bhnid,bhnjd->bhnij', k, k) * decay, 0.0)
    rhs = jnp.concatenate([v * beta[..., None], k * (beta * jnp.exp(gam))[..., None]], axis=-1)
    sol = lax.linalg.triangular_solve(a_mat, rhs, left_side=True, lower=True, unit_diagonal=True)
    u_base, w_kc = sol[..., :dv], sol[..., dv:]
    qk = jnp.einsum('bhnid,bhnjd->bhnij', q, k) * decay
    q_dec = q * jnp.exp(gam)[..., None]
    k_dec = k * jnp.exp(gam[..., -1:] - gam)[..., None]
    chunk_decay = jnp.exp(gam[..., -1])
    xs = tuple(jnp.moveaxis(t, 2, 0) for t in (u_base, w_kc, qk, q_dec, k_dec, chunk_decay))

    def step(state, inp):
        u_b, w_c, qk_c, qd, kd, cd = inp
        u = u_b - jnp.einsum('bhck,bhkv->bhcv', w_c, state)
        o = jnp.einsum('bhck,bhkv->bhcv', qd, state) + jnp.einsum('bhij,bhjv->bhiv', qk_c, u)
        state = state * cd[..., None, None] + jnp.einsum('bhck,bhcv->bhkv', kd, u)
        return state, o

    s0 = jnp.zeros((b, h, dk, dv), jnp.float32)
    _, o = lax.scan(step, s0, xs)
    return jnp.moveaxis(o, 0, 2).reshape(b, h, s, dv)


def gla_chunked(q, k, v, log_a):
    b, h, s, dk = q.shape
    dv = v.shape[-1]
    c = GLA_CHUNK
    n = s // c
    to_chunks = lambda t: jnp.moveaxis(t.reshape(b, h, n, c, t.shape[-1]), 2, 0)
    gam = jnp.cumsum(log_a.reshape(b, h, n, c, dk), axis=3)
    xs = (to_chunks(q), to_chunks(k), to_chunks(v), jnp.moveaxis(gam, 2, 0))
    tri = jnp.tril(jnp.ones((c, c), bool))

    def step(state, inp):
        qc, kc, vc, gc = inp
        diff = gc[:, :, :, None, :] - gc[:, :, None, :, :]
        pair = jnp.exp(jnp.where(tri[:, :, None], diff, -jnp.inf))
        att = jnp.einsum('bhid,bhjd,bhijd->bhij', qc, kc, pair)
        o = (jnp.einsum('bhik,bhkv->bhiv', qc * jnp.exp(gc), state)
             + jnp.einsum('bhij,bhjv->bhiv', att, vc))
        g_last = gc[:, :, -1:, :]
        state = (state * jnp.exp(g_last[:, :, 0, :, None])
                 + jnp.einsum('bhjk,bhjv->bhkv', kc * jnp.exp(g_last - gc), vc))
        return state, o

    s0 = jnp.zeros((b, h, dk, dv), jnp.float32)
    _, o = lax.scan(step, s0, xs)
    return jnp.moveaxis(o, 0, 2).reshape(b, h, s, dv)


def hybrid_layer(x, c_act, ada_w, ada_b, norm_g, w_in, sb_qnorm, sb_knorm, gdn_conv, gdn_a_log,
                 gdn_dt_bias, gdn_onorm, gla_w2, gla_b, gla_onorm, merge_b, proj_sb, proj_gdn,
                 proj_gla, w_out):
    f32 = jnp.float32
    shift, scale, gate = jnp.split(c_act @ ada_w + ada_b, 3, axis=-1)
    h = rms_norm(x, norm_g) * (1 + scale[:, None, :]) + shift[:, None, :]
    offsets = np.cumsum(IN_SPLITS)[:-1].tolist()
    (sq, sk, sv, sz, dq, dk_, dv_, dz, db, da,
     lq, lk, lv, lz, lr, mg) = jnp.split(h @ w_in, offsets, axis=-1)

    q = rms_norm(split_heads(sq, SB_HEADS), sb_qnorm).astype(f32)
    k = rms_norm(split_heads(sk, SB_HEADS), sb_knorm).astype(f32)
    o_sb = stick_breaking_attention(q, k, split_heads(sv, SB_HEADS).astype(f32))
    y_sb = merge_heads(o_sb).astype(x.dtype) * jax.nn.silu(sz)

    qkv = jax.nn.silu(causal_depthwise_conv(jnp.concatenate([dq, dk_, dv_], axis=-1), gdn_conv))
    gq, gk, gv = jnp.split(qkv, 3, axis=-1)
    gq = l2_norm(split_heads(gq, GDN_HEADS)) * (GDN_HD ** -0.5)
    gk = l2_norm(split_heads(gk, GDN_HEADS))
    beta = jax.nn.sigmoid(db.astype(f32)).transpose(0, 2, 1)
    g = (-jnp.exp(gdn_a_log.astype(f32))
         * jax.nn.softplus(da.astype(f32) + gdn_dt_bias.astype(f32))).transpose(0, 2, 1)
    o_gdn = gated_delta_rule(gq, gk, split_heads(gv, GDN_HEADS).astype(f32), beta, g)
    y_gdn = merge_heads(rms_norm(o_gdn, gdn_onorm)).astype(x.dtype) * jax.nn.silu(dz)

    log_a = jax.nn.log_sigmoid((lr @ gla_w2 + gla_b).astype(f32)) / GLA_TAU
    o_gla = gla_chunked(split_heads(lq, GLA_HEADS).astype(f32) * (GLA_KD ** -0.5),
                        split_heads(lk, GLA_HEADS).astype(f32),
                        split_heads(lv, GLA_HEADS).astype(f32),
                        split_heads(log_a, GLA_HEADS))
    y_gla = merge_heads(rms_norm(o_gla, gla_onorm)).astype(x.dtype) * jax.nn.silu(lz)

    g_sb, g_gdn, g_gla = jnp.split(jax.nn.sigmoid(mg + merge_b), 3, axis=-1)
    merged = g_sb * (y_sb @ proj_sb) + g_gdn * (y_gdn @ proj_gdn) + g_gla * (y_gla @ proj_gla)
    return x + gate[:, None, :] * (merged @ w_out)


def setup_inputs(seed: int = 0) -> dict:
    key = jax.random.key(seed)
    ks = jax.random.split(key, 24)
    L, D = DEPTH, D_MODEL
    nrm = lambda k, shape, s: jax.random.normal(k, shape, jnp.float32) * s
    dt = jnp.exp(jax.random.uniform(ks[10], (L, GDN_HEADS), jnp.float32,
                                    minval=math.log(1e-3), maxval=math.log(1e-1)))
    return {
        "x": nrm(ks[0], (BATCH, SEQ, D), 1.0),
        "c": nrm(ks[1], (BATCH, D), 1.0),
        "ada_w": nrm(ks[2], (L, D, 3 * D), 0.5 * D ** -0.5),
        "ada_b": nrm(ks[3], (L, 3 * D), 0.02),
        "norm_g": 1.0 + nrm(ks[4], (L, D), 0.02),
        "w_in": nrm(ks[5], (L, D, N_IN), D ** -0.5),
        "sb_qnorm": 1.0 + nrm(ks[6], (L, SB_HD), 0.02),
        "sb_knorm": 1.0 + nrm(ks[7], (L, SB_HD), 0.02),
        "gdn_conv": nrm(ks[8], (L, GDN_CONV, 3 * GDN_W), GDN_CONV ** -0.5),
        "gdn_a_log": jnp.log(jax.random.uniform(ks[9], (L, GDN_HEADS), jnp.float32,
                                                minval=1.0, maxval=16.0)),
        "gdn_dt_bias": dt + jnp.log(-jnp.expm1(-dt)),
        "gdn_onorm": 1.0 + nrm(ks[11], (L, GDN_HD), 0.02),
        "gla_w2": nrm(ks[12], (L, GLA_RANK, GLA_KW), GLA_RANK ** -0.5),
        "gla_b": nrm(ks[13], (L, GLA_KW), 0.1),
        "gla_onorm": 1.0 + nrm(ks[14], (L, GLA_VD), 0.02),
        "merge_b": nrm(ks[15], (L, N_BRANCH * D), 0.1),
        "proj_sb": nrm(ks[16], (L, SB_W, D), SB_W ** -0.5),
        "proj_gdn": nrm(ks[17], (L, GDN_W, D), GDN_W ** -0.5),
        "proj_gla": nrm(ks[18], (L, GLA_VW, D), GLA_VW ** -0.5),
        "w_out": nrm(ks[19], (L, D, D), D ** -0.5),
    }


def reference(x, c, ada_w, ada_b, norm_g, w_in, sb_qnorm, sb_knorm, gdn_conv, gdn_a_log,
              gdn_dt_bias, gdn_onorm, gla_w2, gla_b, gla_onorm, merge_b, proj_sb, proj_gdn,
              proj_gla, w_out):
    c_act = jax.nn.silu(c)
    for l in range(DEPTH):
        x = hybrid_layer(x, c_act, ada_w[l], ada_b[l], norm_g[l], w_in[l], sb_qnorm[l],
                         sb_knorm[l], gdn_conv[l], gdn_a_log[l], gdn_dt_bias[l], gdn_onorm[l],
                         gla_w2[l], gla_b[l], gla_onorm[l], merge_b[l], proj_sb[l], proj_gdn[l],
                         proj_gla[l], w_out[l])
    return x
```

```python
import contextlib
import numpy as np
import concourse.bass as bass
import concourse.mybir as mybir
from concourse.bass_utils import run_bass_kernel_spmd

F32 = mybir.dt.float32
BF16 = mybir.dt.bfloat16
AF = mybir.ActivationFunctionType
ALU = mybir.AluOpType
CH = 30000
NEG = -30000.0

D = 1024
O_SQ, O_SK, O_SV, O_SZ = 0, 512, 1024, 1536
O_DQ, O_DK, O_DV, O_DZ, O_DB, O_DA = 2048, 2560, 3072, 3584, 4096, 4100
O_LQ, O_LK, O_LV, O_LZ, O_LR, O_MG = 4104, 4616, 5128, 6152, 7176, 7192
N_IN = 10264


class Buf:
    __slots__ = ("name", "w", "r")

    def __init__(self, name):
        self.name = name
        self.w = None
        self.r = {}


class Eng:
    def __init__(self, name, h):
        self.name = name
        self.h = h
        self.sems = []
        self.count = 0
        self.known = {}


class Slot:
    def __init__(self, sid, sem):
        self.id = sid
        self.sem = sem
        self.val = 0


class Prog:
    def __init__(self, nc):
        self.nc = nc
        self.E = {n: Eng(n, h) for n, h in (("pe", nc.tensor), ("act", nc.scalar), ("dve", nc.vector),
                                            ("pool", nc.gpsimd), ("sp", nc.sync))}
        self.slots = []
        self.nins = 0

    def slot(self, name):
        s = Slot(len(self.slots), self.nc.alloc_semaphore(name="dq_" + name))
        self.slots.append(s)
        return s

    def _wait(self, E, tok):
        kind, who, val = tok
        if kind == "e" and who == "pe" and E.name == "pe":
            return
        key = (kind, who)
        if E.known.get(key, 0) >= val:
            return
        E.known[key] = val
        if kind == "e":
            F = self.E[who]
            semi, v = divmod(val - 1, CH)
            E.h.wait_ge(F.sems[semi], v + 1)
        else:
            E.h.wait_ge(self.slots[who].sem, val)
        self.nins += 1

    def _deps(self, E, reads, writes, skip=None):
        for b in reads:
            if b.w is not None and (b.w[0], b.w[1]) != skip:
                self._wait(E, b.w)
        for b in writes:
            if b.w is not None and (b.w[0], b.w[1]) != skip:
                self._wait(E, b.w)
            for t in list(b.r.values()):
                if (t[0], t[1]) != skip:
                    self._wait(E, t)

    def op(self, eng, fn, reads=(), writes=()):
        E = self.E[eng]
        self._deps(E, reads, writes)
        ins = fn()
        E.count += 1
        semi, _ = divmod(E.count - 1, CH)
        while len(E.sems) <= semi:
            E.sems.append(self.nc.alloc_semaphore(name="%s_s%d" % (eng, len(E.sems))))
        ins.then_inc(E.sems[semi], 1)
        self.nins += 1
        tok = ("e", eng, E.count)
        for b in reads:
            b.r[("e", eng)] = tok
        for b in writes:
            b.w = tok
            b.r = {}
        return tok

    def dma(self, out_ap, in_ap, reads, writes, slot, queue="sp"):
        E = self.E[queue]
        self._deps(E, reads, writes, skip=("d", slot.id))
        ins = E.h.dma_start(out=out_ap, in_=in_ap)
        slot.val += 16
        assert slot.val < 60000, "dma slot overflow"
        ins.then_inc(slot.sem, 16)
        self.nins += 1
        tok = ("d", slot.id, slot.val)
        for b in reads:
            b.r[("d", slot.id)] = tok
        for b in writes:
            b.w = tok
            b.r = {}
        return tok

    def barrier(self):
        for E in self.E.values():
            for F in self.E.values():
                if F is not E and F.count > 0 and F.name != "sp":
                    self._wait(E, ("e", F.name, F.count))
            for s in self.slots:
                if s.val > 0:
                    self._wait(E, ("d", s.id, s.val))


def host_consts():
    p = np.arange(128)[:, None]
    f = np.arange(128)[None, :]
    same = (p // 64) == (f // 64)
    c = {}
    c["ident"] = (p == f).astype(np.float32)
    c["ones"] = np.ones((128, 128), np.float32)
    c["utincl"] = ((p <= f) & same).astype(np.float32)
    c["nutincl"] = -c["utincl"]
    c["ltstrict"] = ((p > f) & same).astype(np.float32)
    negm_strict = np.where((p > f) & same, 0.0, NEG).astype(np.float32)
    negmT_incl = np.where((f >= p) & same, 0.0, NEG).astype(np.float32)
    m01T_incl = ((f >= p) & same).astype(np.float32)
    c["negm_strict4"] = np.tile(negm_strict, (1, 4))
    c["negmT_incl4"] = np.tile(negmT_incl, (1, 4))
    c["m01T_incl4"] = np.tile(m01T_incl, (1, 4))
    c["ident4"] = np.tile(c["ident"], (1, 4))
    c["nuincl_sb"] = -(p >= f).astype(np.float32)
    f5 = np.arange(512)[None, :]
    sbm = [((p + 128 * m) < f5) for m in range(4)]
    c["sbmask01"] = np.concatenate([m.astype(np.float32) for m in sbm], axis=1)
    c["sbnegmask"] = np.concatenate([np.where(m, 0.0, NEG).astype(np.float32) for m in sbm], axis=1)
    return c


CONST_ORDER = ["ident", "ones", "utincl", "nutincl", "ltstrict", "negm_strict4", "negmT_incl4", "m01T_incl4"]
CONSTB_ORDER = ["nuincl_sb", "ident4", "sbmask01", "sbnegmask"]


def const_layout():
    c = host_consts()
    off = {}
    o = 0
    for k in CONST_ORDER:
        off[k] = (o, c[k].shape[1])
        o += c[k].shape[1]
    arr = np.concatenate([c[k] for k in CONST_ORDER], axis=1).astype(np.float32)
    arrb = np.concatenate([c[k] for k in CONSTB_ORDER], axis=1).astype(np.float32)
    return arr, off, arrb


def blk(w, cols):
    sub = w[:, cols]
    n = sub.shape[1]
    return np.ascontiguousarray(sub.reshape(8, 128, n).transpose(1, 0, 2).reshape(128, 8 * n))


def fm(v):
    return np.ascontiguousarray(v.reshape(-1, 128).T)


def prep_common(inp, L, NSEQ):
    ar = np.arange
    out = {}
    cst, _, cstb = const_layout()
    out["cst"] = cst
    out["cstb"] = cstb
    w_in = inp["w_in"]
    out["ada_w"] = np.stack([blk(inp["ada_w"][l], ar(3072)).reshape(128, 8, 3072) for l in range(L)])
    out["ada_b2"] = np.ascontiguousarray(np.stack(
        [np.repeat(fm(inp["ada_b"][l])[:, :, None], NSEQ, axis=2) for l in range(L)], axis=1))
    out["norm_g2"] = np.ascontiguousarray(np.stack(
        [np.repeat(fm(inp["norm_g"][l])[:, :, None], NSEQ, axis=2) for l in range(L)], axis=1))
    w_sb = np.empty((L, 4, 128, 4096), np.float32)
    w_gdn = np.empty((L, 4, 128, 4096), np.float32)
    w_gla1 = np.empty((L, 4, 128, 4096), np.float32)
    w_glaz = np.empty((L, 4, 128, 2048), np.float32)
    w_ba = np.empty((L, 128, 64), np.float32)
    w_lr = np.empty((L, 128, 128), np.float32)
    w_mg = np.empty((L, 8, 128, 3072), np.float32)
    w_pj = np.empty((L, 8, 128, 2048), np.float32)
    w_o = np.empty((L, 8, 128, 1024), np.float32)
    for l in range(L):
        w = w_in[l]
        for h in range(4):
            a = ar(128) + 128 * h
            w_sb[l, h] = blk(w, np.concatenate([O_SQ + a, O_SK + a, O_SV + a, O_SZ + a]))
            w_gdn[l, h] = blk(w, np.concatenate([O_DQ + a, O_DK + a, O_DV + a, O_DZ + a]))
            a2 = ar(256) + 256 * h
            w_gla1[l, h] = blk(w, np.concatenate([O_LQ + a, O_LK + a, O_LV + a2]))
            w_glaz[l, h] = blk(w, O_LZ + a2)
        w_ba[l] = blk(w, O_DB + ar(8))
        w_lr[l] = blk(w, O_LR + ar(16))
        for nb in range(8):
            a = ar(128) + 128 * nb
            w_mg[l, nb] = blk(w, np.concatenate([O_MG + a, O_MG + 1024 + a, O_MG + 2048 + a]))
            psb = inp["proj_sb"][l][:, a].reshape(4, 128, 128).transpose(1, 0, 2).reshape(128, 512)
            pgd = inp["proj_gdn"][l][:, a].reshape(4, 128, 128).transpose(1, 0, 2).reshape(128, 512)
            pgl = inp["proj_gla"][l][:, a].reshape(8, 128, 128).transpose(1, 0, 2).reshape(128, 1024)
            w_pj[l, nb] = np.concatenate([psb, pgd, pgl], axis=1)
            w_o[l, nb] = inp["w_out"][l][:, a].reshape(8, 128, 128).transpose(1, 0, 2).reshape(128, 1024)
    out.update(w_sb=w_sb, w_gdn=w_gdn, w_gla1=w_gla1, w_glaz=w_glaz, w_ba=w_ba, w_lr=w_lr, w_mg=w_mg,
               w_pj=w_pj, w_o=w_o)
    sp = {}
    sp["qn"] = np.stack([inp["sb_qnorm"][l] for l in range(L)], axis=1)
    sp["kn"] = np.stack([inp["sb_knorm"][l] for l in range(L)], axis=1)
    sp["conv"] = np.stack([inp["gdn_conv"][l].T.reshape(12, 128, 4).transpose(1, 0, 2).reshape(128, 48)
                           for l in range(L)], axis=1).reshape(128, L * 48)
    sp["gon"] = np.stack([inp["gdn_onorm"][l] for l in range(L)], axis=1)
    sp["lon"] = np.stack([fm(inp["gla_onorm"][l]) for l in range(L)], axis=1).reshape(128, L * 2)
    sp["mb"] = np.stack([fm(inp["merge_b"][l]) for l in range(L)], axis=1).reshape(128, L * 24)
    sp["alog"] = np.tile(np.stack([inp["gdn_a_log"][l] for l in range(L)], 0).reshape(1, L * 4), (128, 1))
    sp["dtb"] = np.tile(np.stack([inp["gdn_dt_bias"][l] for l in range(L)], 0).reshape(1, L * 4), (128, 1))
    order = ["qn", "kn", "conv", "gon", "lon", "mb", "alog", "dtb"]
    out["ada_b2"] = out["ada_b2"].reshape(128, -1)
    out["norm_g2"] = out["norm_g2"].reshape(128, -1)
    out["spar"] = np.ascontiguousarray(np.concatenate([sp[k].astype(np.float32) for k in order], axis=1))
    out["gla_w2"] = np.ascontiguousarray(inp["gla_w2"][:L].astype(np.float32))
    out["gla_b"] = np.ascontiguousarray(inp["gla_b"][:L].reshape(L, 1, 512).astype(np.float32))
    return out


def spar_offsets(L):
    sizes = [("qn", L), ("kn", L), ("conv", L * 48), ("gon", L), ("lon", L * 2), ("mb", L * 24),
             ("alog", L * 4), ("dtb", L * 4)]
    off = {}
    o = 0
    for k, n in sizes:
        off[k] = o
        o += n
    return off, o


def build_program(NSEQ, S, L, dbg=()):
    nc = bass.Bass("TRN2", target_bir_lowering=False)
    P = Prog(nc)
    NTB = S // 512
    NT = S // 128
    cst_np, coff, cstb_np = const_layout()
    NCST = cst_np.shape[1]
    spo, NSP = spar_offsets(L)

    def din(name, shape, dt=F32):
        return nc.dram_tensor(name, list(shape), dt, kind="ExternalInput").ap()

    xT_d = din("xT", [NSEQ, D, S])
    cT_d = din("cT", [128, 8 * NSEQ])
    cst_d = din("cst", [128, NCST])
    cstb_d = din("cstb", [128, cstb_np.shape[1]])
    adaw_d = din("ada_w", [L, 128, 8, 3072])
    adab_d = din("ada_b2", [128, L * 24 * NSEQ])
    ng_d = din("norm_g2", [128, L * 8 * NSEQ])
    wsb_d = din("w_sb", [L, 4, 128, 4096])
    wgdn_d = din("w_gdn", [L, 4, 128, 4096])
    wgla1_d = din("w_gla1", [L, 4, 128, 4096])
    wglaz_d = din("w_glaz", [L, 4, 128, 2048])
    wba_d = din("w_ba", [L, 128, 64])
    wlr_d = din("w_lr", [L, 128, 128])
    wmg_d = din("w_mg", [L, 8, 128, 3072])
    wpj_d = din("w_pj", [L, 8, 128, 2048])
    wo_d = din("w_o", [L, 8, 128, 1024])
    spar_d = din("spar", [128, NSP])
    w2_d = din("gla_w2", [L, 16, 512])
    glab_d = din("gla_b", [L, 1, 512])
    outT_d = nc.dram_tensor("outT", [NSEQ, D, S], F32, kind="ExternalOutput").ap()
    xs_d = nc.dram_tensor("xs", [NSEQ, D, S], F32, kind="Internal").ap()
    dbg_d = {}
    for name, shape in dbg:
        dbg_d[name] = nc.dram_tensor("dbg_" + name, list(shape), F32, kind="ExternalOutput").ap()

    es = contextlib.ExitStack()

    uid = [0]

    def sb(name, shape, dt=F32, stack=None):
        uid[0] += 1
        return (stack or es).enter_context(nc.sbuf_tensor("%s_u%d" % (name, uid[0]), list(shape), dt))

    with es:
        cst = sb("cst", [128, NCST])
        cst_b = Buf("cst")
        identb = sb("identb", [128, 128], BF16)
        onesb = sb("onesb", [128, 128], BF16)
        nuinclb = sb("nuinclb", [128, 128], BF16)
        sbnegb = sb("sbnegb", [128, 2048], BF16)
        sbm01b = sb("sbm01b", [128, 2048], BF16)
        ident4b = sb("ident4b", [128, 512], BF16)
        cb_b = Buf("cstb")
        spar = sb("spar", [128, NSP])
        spar_b = Buf("spar")
        cact = sb("cact", [128, 8 * NSEQ])
        cact_b = Buf("cact")
        adab = sb("adab", [128, L * 24 * NSEQ])
        ng2 = sb("ng2", [128, L * 8 * NSEQ])
        modT = sb("modT", [128, L * 24 * NSEQ])
        Amod = sb("Amod", [128, L * 8 * NSEQ])
        mod_b = Buf("mod")
        dervp = sb("dervp", [128, 5 * L + 8])
        derv_b = Buf("derv")
        hT = sb("hT", [128, 8, S], BF16)
        hT_b = [Buf("hT%d" % t) for t in range(NTB)]
        ysb = sb("ysb", [128, 4, S], BF16)
        ygdn = sb("ygdn", [128, 4, S], BF16)
        ygla = sb("ygla", [128, 8, S], BF16)
        ysb_b = [Buf("ysb%d" % h) for h in range(4)]
        ygdn_b = [Buf("ygdn%d" % h) for h in range(4)]
        ygla_b = [Buf("ygla%d" % h) for h in range(4)]
        NRING, PF = 4, 2
        ring = [sb("ring%d" % i, [128, 4096], BF16) for i in range(NRING)]
        ring_b = [Buf("ring%d" % i) for i in range(NRING)]
        ring_s = [P.slot("ring%d" % i) for i in range(NRING)]
        wsched = []
        for b_ in range(NSEQ):
            for l_ in range(L):
                for h_ in range(4):
                    wsched.append((("sb", l_, h_), wsb_d[l_, h_], 4096))
                wsched.append((("ba", l_), wba_d[l_], 64))
                for h_ in range(4):
                    wsched.append((("gdn", l_, h_), wgdn_d[l_, h_], 4096))
                wsched.append((("lr", l_), wlr_d[l_], 128))
                for h_ in range(4):
                    wsched.append((("gla1", l_, h_), wgla1_d[l_, h_], 4096))
                    wsched.append((("glaz", l_, h_), wglaz_d[l_, h_], 2048))
                for n_ in range(8):
                    wsched.append((("mg", l_, n_), wmg_d[l_, n_], 3072))
                    wsched.append((("pj", l_, n_), wpj_d[l_, n_], 2048))
                for n_ in range(8):
                    wsched.append((("wo", l_, n_), wo_d[l_, n_], 1024))
        wstate = {"next": 0, "issued": 0}
        PS = [es.enter_context(nc.psum_tensor("ps%d" % i, [128, 512], F32)) for i in range(8)]
        PS_b = [Buf("ps%d" % i) for i in range(8)]
        misc_s = P.slot("misc")
        p1_slots = [P.slot("p1x%d" % i) for i in range(2)]
        p5_slots = [P.slot("p5x%d" % i) for i in range(2)]
        misc_b = Buf("miscload")

        def C(name, a=0, b=None):
            o, n = coff[name]
            return cst[:, o + a: o + (n if b is None else b)]

        def SPc(name, i, n=1):
            o = spo[name] + i
            return spar[:, o:o + n]

        P.dma(cst[:], cst_d[:, :], [], [cst_b], misc_s)
        P.dma(spar[:], spar_d[:, :], [], [spar_b], misc_s)
        P.dma(cact[:], cT_d[:, :], [], [cact_b], misc_s)
        P.dma(adab[:], adab_d[:, :], [], [misc_b], misc_s)
        P.dma(ng2[:], ng_d[:, :], [], [misc_b], misc_s)
        P.op("dve", lambda: nc.vector.tensor_copy(identb[:], C("ident")), [cst_b], [cb_b])
        P.op("dve", lambda: nc.vector.tensor_copy(onesb[:], C("ones")), [cst_b], [cb_b])
        stg_es = contextlib.ExitStack()
        stage = [sb("stage%d" % i, [128, 4096], F32, stg_es) for i in range(2)]
        stage_b = [Buf("stage%d" % i) for i in range(2)]
        stage_s = [P.slot("stage%d" % i) for i in range(2)]
        P.dma(stage[0][:, 0:2688], cstb_d[:, 0:2688], [], [stage_b[0]], stage_s[0])
        P.op("dve", lambda: nc.vector.tensor_copy(nuinclb[:], stage[0][:, 0:128]), [stage_b[0]], [cb_b])
        P.op("dve", lambda: nc.vector.tensor_copy(ident4b[:], stage[0][:, 128:640]), [stage_b[0]], [cb_b])
        P.op("dve", lambda: nc.vector.tensor_copy(sbm01b[:], stage[0][:, 640:2688]), [stage_b[0]], [cb_b])
        P.dma(stage[1][:, 0:2048], cstb_d[:, 2688:4736], [], [stage_b[1]], stage_s[1])
        P.op("dve", lambda: nc.vector.tensor_copy(sbnegb[:], stage[1][:, 0:2048]), [stage_b[1]], [cb_b])
        P.op("act", lambda: nc.scalar.activation(out=cact[:], in_=cact[:], func=AF.Silu), [cact_b], [cact_b])
        P.op("dve", lambda: nc.vector.tensor_scalar(dervp[:, 0:L], SPc("qn", 0, L), 128.0 ** -0.5, None, ALU.mult),
             [spar_b], [derv_b])

        P.op("act", lambda: nc.scalar.activation(out=dervp[:, L:L + 4 * L], in_=SPc("alog", 0, 4 * L), func=AF.Exp),
             [spar_b, derv_b], [derv_b])
        P.op("dve", lambda: nc.vector.tensor_scalar(dervp[:, L:L + 4 * L], dervp[:, L:L + 4 * L], -1.0, None, ALU.mult),
             [derv_b], [derv_b])

        def load_w(key):
            i = wstate["next"]
            wstate["next"] += 1
            assert wsched[i][0] == key, (wsched[i][0], key)
            while wstate["issued"] < min(len(wsched), i + PF + 1):
                j = wstate["issued"]
                _, src, n = wsched[j]
                P.dma(ring[j % NRING][:, 0:n], src, [], [ring_b[j % NRING]], ring_s[j % NRING], queue="pool")
                wstate["issued"] += 1
            return ring[i % NRING], ring_b[i % NRING]

        def phase0():
            for l in range(L):
                ps, psb = PS[0], PS_b[0]
                for cb in range(6):
                    si = (l * 6 + cb) % 2
                    stg, stg_b = stage[si], stage_b[si]
                    P.dma(stg[:, 0:4096].rearrange("p (k c) -> p k c", k=8),
                          adaw_d[l, :, :, cb * 512:(cb + 1) * 512], [], [stg_b], stage_s[si])

                    def mm(cb=cb):
                        ins = None
                        for nb in range(4):
                            j = cb * 4 + nb
                            for kc in range(8):
                                ins = nc.tensor.matmul(ps[:, j * NSEQ:(j + 1) * NSEQ],
                                                       stg[:, kc * 512 + nb * 128: kc * 512 + nb * 128 + 128],
                                                       cact[:, kc * NSEQ:(kc + 1) * NSEQ],
                                                       start=(kc == 0), stop=(kc == 7))
                        return ins
                    P.op("pe", mm, [stg_b, cact_b], [psb])
                o = l * 24 * NSEQ
                P.op("dve", lambda o=o: nc.vector.tensor_tensor(modT[:, o:o + 24 * NSEQ], ps[:, 0:24 * NSEQ],
                                                                adab[:, o:o + 24 * NSEQ], ALU.add),
                     [psb, misc_b], [mod_b])
                oa = l * 8 * NSEQ
                P.op("dve", lambda o=o, oa=oa: nc.vector.scalar_tensor_tensor(
                    Amod[:, oa:oa + 8 * NSEQ], modT[:, o + 8 * NSEQ:o + 16 * NSEQ], 1.0, ng2[:, oa:oa + 8 * NSEQ],
                    ALU.add, ALU.mult), [mod_b, misc_b], [mod_b])

        def m_shift(l, j, b):
            o = (l * 24 + j) * NSEQ + b
            return modT[:, o:o + 1]

        def m_gate(l, j, b):
            o = (l * 24 + 16 + j) * NSEQ + b
            return modT[:, o:o + 1]

        def m_A(l, j, b):
            o = (l * 8 + j) * NSEQ + b
            return Amod[:, o:o + 1]

        def xsrc(l, b):
            return (xT_d if l == 0 else xs_d)[b].rearrange("(k p) t -> p k t", p=128)

        def xdst(l, b):
            return (outT_d if l == L - 1 else xs_d)[b].rearrange("(k p) t -> p k t", p=128)

        xs_b = [[Buf("xs%d_%d" % (b, t)) for t in range(NTB)] for b in range(NSEQ)]

        def phase1(l, b):
            with contextlib.ExitStack() as ph:
                xt = [sb("p1x%d" % i, [128, 8, 512], F32, ph) for i in range(2)]
                xt_b = [Buf("p1x%d" % i) for i in range(2)]
                xt_s = p1_slots
                sq = [sb("p1sq%d" % i, [128, 512], F32, ph) for i in range(2)]
                sq_b = [Buf("p1sq%d" % i) for i in range(2)]
                tmp = [sb("p1t%d" % i, [128, 512], F32, ph) for i in range(2)]
                tmp_b = [Buf("p1t%d" % i) for i in range(2)]
                srt = sb("p1srt", [128, 512], F32, ph)
                rstd = sb("p1rstd", [128, 512], F32, ph)
                srt_b, rstd_b = Buf("srt"), Buf("rstd")
                src = xsrc(l, b)
                for tb in range(NTB):
                    i = tb % 2
                    P.dma(xt[i][:], src[:, :, tb * 512:(tb + 1) * 512], [xs_b[b][tb]], [xt_b[i]], xt_s[i])
                    ps, psb = PS[tb % 2], PS_b[tb % 2]
                    for j in range(8):
                        k = j % 2
                        P.op("act", lambda j=j, k=k: nc.scalar.activation(out=sq[k][:], in_=xt[i][:, j, :],
                                                                          func=AF.Square), [xt_b[i]], [sq_b[k]])
                        P.op("pe", lambda j=j, k=k: nc.tensor.matmul(ps[:], C("ones"), sq[k][:], start=(j == 0),
                                                                     stop=(j == 7)), [sq_b[k], cst_b], [psb])
                    P.op("act", lambda: nc.scalar.activation(out=srt[:], in_=ps[:], func=AF.Sqrt, bias=1e-6,
                                                             scale=1.0 / D), [psb], [srt_b])
                    P.op("dve", lambda: nc.vector.reciprocal(rstd[:], srt[:]), [srt_b], [rstd_b])
                    for j in range(8):
                        k = j % 2
                        P.op("dve", lambda j=j, k=k: nc.vector.tensor_tensor(tmp[k][:], xt[i][:, j, :], rstd[:],
                                                                             ALU.mult), [xt_b[i], rstd_b], [tmp_b[k]])
                        P.op("act", lambda j=j, k=k: nc.scalar.activation(
                            out=hT[:, j, tb * 512:(tb + 1) * 512], in_=tmp[k][:], func=AF.Identity,
                            bias=m_shift(l, j, b), scale=m_A(l, j, b)), [tmp_b[k], mod_b], [hT_b[tb]])
                P.barrier()

        def phase5(l, b):
            with contextlib.ExitStack() as ph:
                mrg = sb("p5mrg", [128, 8, S], BF16, ph)
                mrg_b = [Buf("mrg%d" % t) for t in range(NTB)]
                g = [sb("p5g%d" % i, [128, 512], F32, ph) for i in range(3)]
                g_b = [Buf("p5g%d" % i) for i in range(3)]
                t_ = [sb("p5t%d" % i, [128, 512], F32, ph) for i in range(3)]
                t_b = [Buf("p5t%d" % i) for i in range(3)]
                xr = [sb("p5x%d" % i, [128, 512], F32, ph) for i in range(2)]
                xr_b = [Buf("p5x%d" % i) for i in range(2)]
                xr_s = p5_slots
                src = xsrc(l, b)
                dst = xdst(l, b)
                for nb in range(8):
                    mgw, mgw_b = load_w(("mg", l, nb))
                    pj, pj_b = load_w(("pj", l, nb))
                    for tb in range(NTB):
                        tok = slice(tb * 512, (tb + 1) * 512)
                        for br in range(3):
                            def mmg(br=br):
                                ins = None
                                for kc in range(8):
                                    ins = nc.tensor.matmul(PS[br][:], mgw[:, kc * 384 + br * 128: kc * 384 + br * 128 + 128],
                                                           hT[:, kc, tok], start=(kc == 0), stop=(kc == 7))
                                return ins
                            P.op("pe", mmg, [mgw_b, hT_b[tb]], [PS_b[br]])
                            P.op("act", lambda br=br: nc.scalar.activation(
                                out=g[br][:], in_=PS[br][:], func=AF.Sigmoid,
                                bias=SPc("mb", l * 24 + br * 8 + nb)), [PS_b[br], spar_b], [g_b[br]])
                        ysrc = [(ysb, ysb_b, 4, 0), (ygdn, ygdn_b, 4, 512), (ygla, ygla_b, 8, 1024)]
                        for br in range(3):
                            yt, yb, nk, wo = ysrc[br]

                            def mmp(yt=yt, nk=nk, wo=wo, br=br):
                                ins = None
                                for kc in range(nk):
                                    ins = nc.tensor.matmul(PS[3 + br][:], pj[:, wo + kc * 128: wo + kc * 128 + 128],
                                                           yt[:, kc, tok], start=(kc == 0), stop=(kc == nk - 1))
                                return ins
                            P.op("pe", mmp, [pj_b] + yb, [PS_b[3 + br]])
                            P.op("dve", lambda br=br: nc.vector.tensor_tensor(t_[br][:], PS[3 + br][:], g[br][:],
                                                                             ALU.mult), [PS_b[3 + br], g_b[br]], [t_b[br]])
                        P.op("pool", lambda: nc.gpsimd.tensor_tensor(t_[0][:], t_[0][:], t_[1][:], ALU.add),
                             [t_b[0], t_b[1]], [t_b[0]])
                        P.op("pool", lambda: nc.gpsimd.tensor_tensor(mrg[:, nb, tok], t_[0][:], t_[2][:], ALU.add),
                             [t_b[0], t_b[2]], [mrg_b[tb]])
                for nb in range(8):
                    wo_t, wo_b = load_w(("wo", l, nb))
                    for tb in range(NTB):
                        tok = slice(tb * 512, (tb + 1) * 512)
                        i = (nb * NTB + tb) % 2
                        ps, psb = PS[6 + i], PS_b[6 + i]
                        P.dma(xr[i][:], src[:, nb, tok], [xs_b[b][tb]], [xr_b[i]], xr_s[i])

                        def mmo():
                            ins = None
                            for kc in range(8):
                                ins = nc.tensor.matmul(ps[:], wo_t[:, kc * 128:(kc + 1) * 128], mrg[:, kc, tok],
                                                       start=(kc == 0), stop=(kc == 7))
                            return ins
                        P.op("pe", mmo, [wo_b, mrg_b[tb]], [psb])
                        P.op("dve", lambda: nc.vector.scalar_tensor_tensor(xr[i][:], ps[:], m_gate(l, nb, b), xr[i][:],
                                                                           ALU.mult, ALU.add), [psb, xr_b[i], mod_b], [xr_b[i]])
                        P.dma(dst[:, nb, tok], xr[i][:], [xr_b[i]], [xo_b[b][tb]], xr_s[i])
                P.barrier()

        xo_b = [[Buf("xo%d_%d" % (b, t)) for t in range(NTB)] for b in range(NSEQ)]

        build_mixers = MIXERS
        ctx = dict(nc=nc, P=P, sb=sb, C=C, SPc=SPc, PS=PS, PS_b=PS_b, hT=hT, hT_b=hT_b, ysb=ysb, ygdn=ygdn,
                   ygla=ygla, ysb_b=ysb_b, ygdn_b=ygdn_b, ygla_b=ygla_b, load_w=load_w, cst_b=cst_b, cb_b=cb_b,
                   spar_b=spar_b, identb=identb, onesb=onesb, nuinclb=nuinclb, sbnegb=sbnegb, sbm01b=sbm01b,
                   ident4b=ident4b, dervp=dervp, derv_b=derv_b, S=S, L=L, NTB=NTB, NT=NT, NSEQ=NSEQ,
                   wsb_d=wsb_d, wgdn_d=wgdn_d, wgla1_d=wgla1_d, wglaz_d=wglaz_d, wba_d=wba_d, wlr_d=wlr_d,
                   w2_d=w2_d, glab_d=glab_d, dbg_d=dbg_d,
                   misc_s=misc_s, spo=spo, spar=spar)

        phase0()
        P.barrier()
        stg_es.close()
        for b in range(NSEQ):
            for l in range(L):
                phase1(l, b)
                for m in build_mixers:
                    m(ctx, l, b)
                phase5(l, b)
                for t in range(NTB):
                    xs_b[b][t].w = xo_b[b][t].w
                    xs_b[b][t].r = {}
                    xo_b[b][t].w = None
        P.barrier()
    return nc, P


def mixer_sb(ctx, l, b):
    g = ctx
    nc, P, sb, C, SPc, PS, PS_b = g["nc"], g["P"], g["sb"], g["C"], g["SPc"], g["PS"], g["PS_b"]
    hT, hT_b, S, NTB, NT = g["hT"], g["hT_b"], g["S"], g["NTB"], g["NT"]
    ysb, ysb_b = g["ysb"], g["ysb_b"]
    cst_b, cb_b, spar_b, derv_b = g["cst_b"], g["cb_b"], g["spar_b"], g["derv_b"]
    with contextlib.ExitStack() as ph:
        def T(name, shape, dt=F32):
            return sb("s" + name, shape, dt, ph), Buf("s" + name)
        qT, q_b = T("q", [128, S], BF16)
        kT, k_b = T("k", [128, S], BF16)
        zg, z_b = T("z", [128, S], BF16)
        v, v_b = T("v", [128, NT, 128], BF16)
        ST = []
        for si in range(2):
            ST.append(dict(
                esp=[T("esp%d_%d" % (si, i), [128, 512]) for i in range(2)],
                sph=[T("sph%d_%d" % (si, i), [128, 512], BF16) for i in range(2)],
                spl=[T("spl%d_%d" % (si, i), [128, 512], BF16) for i in range(2)],
                arg=T("arg%d" % si, [128, 512]),
                W=[T("W%d_%d" % (si, i), [128, 512], BF16) for i in range(2)],
                C=[T("C%d_%d" % (si, i), [128, 512]) for i in range(2)],
                za=[(PS[2 * si], PS_b[2 * si]), (PS[2 * si + 1], PS_b[2 * si + 1])],
                o=(PS[4 + 2 * si], PS_b[4 + 2 * si]), c=(PS[5 + 2 * si], PS_b[5 + 2 * si])))
        tsets = [(ST[si]["esp"][0], ST[si]["esp"][1], ST[si]["arg"]) for si in range(2)]
        for hd in range(4):
            w, w_b = g["load_w"](("sb", l, hd))
            n = 0
            for which in range(2):
                dst, dst_b = (qT, q_b) if which == 0 else (kT, k_b)
                gain = g["dervp"][:, l:l + 1] if which == 0 else SPc("kn", l)
                for tb in range(NTB):
                    tok = slice(tb * 512, (tb + 1) * 512)
                    ps, psb = PS[6 + n % 2], PS_b[6 + n % 2]
                    ps2, ps2b = PS[4 + n % 2], PS_b[4 + n % 2]
                    (sqt, sq_b), (srt, srt_b), (rr, rr_b) = tsets[n % 2]
                    n += 1

                    def mm():
                        ins = None
                        for kc in range(8):
                            ins = nc.tensor.matmul(ps[:], w[:, kc * 512 + which * 128: kc * 512 + which * 128 + 128],
                                                   hT[:, kc, tok], start=(kc == 0), stop=(kc == 7))
                        return ins
                    P.op("pe", mm, [w_b, hT_b[tb]], [psb])
                    P.op("act", lambda: nc.scalar.activation(out=sqt[:], in_=ps[:], func=AF.Square), [psb], [sq_b])
                    P.op("pe", lambda: nc.tensor.matmul(ps2[:], C("ones"), sqt[:], start=True, stop=True),
                         [sq_b, cst_b], [ps2b])
                    P.op("act", lambda: nc.scalar.activation(out=srt[:], in_=ps2[:], func=AF.Sqrt, bias=1e-6,
                                                             scale=1.0 / 128), [ps2b], [srt_b])
                    P.op("dve", lambda: nc.vector.reciprocal(rr[:], srt[:]), [srt_b], [rr_b])
                    P.op("dve", lambda: nc.vector.scalar_tensor_tensor(
                        dst[:, tok], ps[:], gain, rr[:], ALU.mult, ALU.mult), [psb, rr_b, derv_b, spar_b], [dst_b])
            for tb in range(NTB):
                tok = slice(tb * 512, (tb + 1) * 512)
                ps, psb = PS[6 + tb % 2], PS_b[6 + tb % 2]

                def mmz():
                    ins = None
                    for kc in range(8):
                        ins = nc.tensor.matmul(ps[:], w[:, kc * 512 + 384: kc * 512 + 512], hT[:, kc, tok],
                                               start=(kc == 0), stop=(kc == 7))
                    return ins
                P.op("pe", mmz, [w_b, hT_b[tb]], [psb])
                P.op("act", lambda: nc.scalar.activation(out=zg[:, tok], in_=ps[:], func=AF.Silu), [psb], [z_b])
            for t4 in range(NT // 4):
                ps, psb = PS[t4 % 2], PS_b[t4 % 2]

                def mmv():
                    ins = None
                    for tt in range(4):
                        t = t4 * 4 + tt
                        for kc in range(8):
                            ins = nc.tensor.matmul(ps[:, tt * 128:(tt + 1) * 128], hT[:, kc, t * 128:(t + 1) * 128],
                                                   w[:, kc * 512 + 256: kc * 512 + 384], start=(kc == 0), stop=(kc == 7))
                    return ins
                P.op("pe", mmv, [w_b, hT_b[t4]], [psb])
                P.op("act", lambda: nc.scalar.copy(v[:, t4 * 4:(t4 + 1) * 4, :], ps[:].rearrange("p (a c) -> p a c", a=4)),
                     [psb], [v_b])
            qts = [[], []]
            order = list(reversed(range(NTB)))
            for k_, qt in enumerate(order):
                qts[0 if (k_ % 4) in (0, 3) else 1].append(qt)
            tiles = [[(qt, kb) for qt in qts[si] for kb in reversed(range(4 * qt + 4))] for si in range(2)]

            def front(si, i):
                st = ST[si]
                qt, kb = tiles[si][i]
                m = kb - 4 * qt
                qtok = slice(qt * 512, (qt + 1) * 512)
                ktok = slice(kb * 128, (kb + 1) * 128)
                (za, za_b) = st["za"][i % 2]
                (esp, esp_b) = st["esp"][i % 2]
                (sph, sph_b) = st["sph"][i % 2]
                (spl, spl_b) = st["spl"][i % 2]
                P.op("pe", lambda: nc.tensor.matmul(za[:], kT[:, ktok], qT[:, qtok], start=True, stop=True), [k_b, q_b], [za_b])
                P.op("act", lambda: nc.scalar.activation(out=esp[:], in_=za[:], func=AF.Exp), [za_b], [esp_b])
                P.op("act", lambda: nc.scalar.activation(out=esp[:], in_=esp[:], func=AF.Ln, bias=1.0), [esp_b], [esp_b])
                if m >= 0:
                    P.op("pool", lambda: nc.gpsimd.tensor_tensor(esp[:], esp[:], g["sbm01b"][:, m * 512:(m + 1) * 512], ALU.mult),
                         [esp_b, cb_b], [esp_b])
                P.op("dve", lambda: nc.vector.tensor_copy(sph[:], esp[:]), [esp_b], [sph_b])
                P.op("pool", lambda: nc.gpsimd.tensor_tensor(spl[:], esp[:], sph[:], ALU.subtract), [esp_b, sph_b], [spl_b])

            def back(si, i):
                st = ST[si]
                qt, kb = tiles[si][i]
                m = kb - 4 * qt
                first = (kb == 4 * qt + 3)
                qtok = slice(qt * 512, (qt + 1) * 512)
                ktok = slice(kb * 128, (kb + 1) * 128)
                (za, za_b) = st["za"][i % 2]
                (sph, sph_b) = st["sph"][i % 2]
                (spl, spl_b) = st["spl"][i % 2]
                (arg, arg_b) = st["arg"]
                (W, W_b) = st["W"][i % 2]
                (Cc, Cc_b) = st["C"][i % 2]
                (Cp, Cp_b) = st["C"][(i - 1) % 2]
                (ops_, ops_b) = st["o"]
                (cps, cps_b) = st["c"]

                def mma():
                    nc.tensor.matmul(za[:], kT[:, ktok], qT[:, qtok], start=True, stop=False)
                    nc.tensor.matmul(za[:], g["nuinclb"][:], sph[:], start=False, stop=False)
                    ins = nc.tensor.matmul(za[:], g["nuinclb"][:], spl[:], start=False, stop=(m < 0))
                    if m >= 0:
                        ins = nc.tensor.matmul(za[:], g["identb"][:], g["sbnegb"][:, m * 512:(m + 1) * 512], start=False, stop=True)
                    return ins
                P.op("pe", mma, [k_b, q_b, sph_b, spl_b, cb_b], [za_b])
                if kb > 0:
                    def mmc():
                        nc.tensor.matmul(cps[:], g["onesb"][:], sph[:], start=first, stop=False)
                        return nc.tensor.matmul(cps[:], g["onesb"][:], spl[:], start=False, stop=True)
                    P.op("pe", mmc, [sph_b, spl_b, cb_b], [cps_b])
                if first:
                    P.op("act", lambda: nc.scalar.activation(out=W[:], in_=za[:], func=AF.Exp), [za_b], [W_b])
                else:
                    P.op("dve", lambda: nc.vector.tensor_tensor(arg[:], za[:], Cp[:], ALU.subtract), [za_b, Cp_b], [arg_b])
                    P.op("act", lambda: nc.scalar.activation(out=W[:], in_=arg[:], func=AF.Exp), [arg_b], [W_b])
                if kb > 0:
                    P.op("dve", lambda: nc.vector.tensor_copy(Cc[:], cps[:]), [cps_b], [Cc_b])
                P.op("pe", lambda: nc.tensor.matmul(ops_[:], v[:, kb, :], W[:], start=first, stop=(kb == 0)), [v_b, W_b], [ops_b])
                if kb == 0:
                    P.op("dve", lambda: nc.vector.tensor_tensor(ysb[:, hd, qtok], ops_[:], zg[:, qtok], ALU.mult),
                         [ops_b, z_b], [ysb_b[hd]])

            nmax = max(len(t_) for t_ in tiles)
            for si in range(2):
                if len(tiles[si]) > 0:
                    front(si, 0)
            for i in range(nmax):
                for si in range(2):
                    if i + 1 < len(tiles[si]):
                        front(si, i + 1)
                for si in range(2):
                    if i < len(tiles[si]):
                        back(si, i)
        P.barrier()


def mixer_gla(ctx, l, b):
    g = ctx
    nc, P, sb, C, SPc, PS, PS_b = g["nc"], g["P"], g["sb"], g["C"], g["SPc"], g["PS"], g["PS_b"]
    hT, hT_b, S, NTB, NT = g["hT"], g["hT_b"], g["S"], g["NTB"], g["NT"]
    ygla, ygla_b = g["ygla"], g["ygla_b"]
    cst_b, cb_b, spar_b = g["cst_b"], g["cb_b"], g["spar_b"]
    identb, onesb = g["identb"], g["onesb"]
    with contextlib.ExitStack() as ph:
        lrT = sb("glrT", [16, S], BF16, ph)
        lr_b = Buf("lrT")
        w2 = sb("gw2", [16, 512], BF16, ph)
        glab = sb("gglab", [1, 512], BF16, ph)
        w2f = sb("gw2f", [16, 512], F32, ph)
        glabf = sb("gglabf", [1, 512], F32, ph)
        w2_b = Buf("w2")
        qg = sb("gqg", [128, 512], BF16, ph)
        kg = sb("gkg", [128, 512], BF16, ph)
        kb16 = sb("gkb", [128, 512], BF16, ph)
        qk_b = Buf("gqk")
        la = sb("gla", [128, 512], F32, ph)
        la_b = Buf("la")
        ex = sb("gex", [128, 512], F32, ph)
        ex_b = Buf("gex")
        eg = sb("geg", [128, 512], F32, ph)
        eng = sb("geng", [128, 512], F32, ph)
        eg_b = Buf("eg")
        edk = sb("gedk", [128, 512], F32, ph)
        edk_b = Buf("edk")
        kd = sb("gkd", [128, 4, 128], BF16, ph)
        kd_b = Buf("kd")
        att = sb("gatt", [128, 4, 128], BF16, ph)
        att_b = Buf("att")
        v = sb("gv", [128, 4, 256], BF16, ph)
        v_b = Buf("gv")
        zg = sb("gzg", [128, 2, 512], BF16, ph)
        zg_b = Buf("gzg")
        St = sb("gS", [128, 256], F32, ph)
        Sb = sb("gSb", [128, 256], BF16, ph)
        S_b, Sb_b = Buf("gS"), Buf("gSb")
        sq = sb("gsq", [128, 512], F32, ph)
        srt = sb("gsrt", [128, 512], F32, ph)
        rr = sb("grr", [128, 512], F32, ph)
        tmp = sb("gtmp", [128, 512], F32, ph)
        sq_b, srt_b, rr_b, tmp_b = Buf("gsq"), Buf("gsrt"), Buf("grr"), Buf("gtmp")
        P.dma(w2f[:], g["w2_d"][l], [], [w2_b], g["misc_s"])
        P.dma(glabf[:], g["glab_d"][l], [], [w2_b], g["misc_s"])
        P.op("dve", lambda: nc.vector.tensor_copy(w2[:], w2f[:]), [w2_b], [w2_b])
        P.op("dve", lambda: nc.vector.tensor_copy(glab[:], glabf[:]), [w2_b], [w2_b])
        wl, wl_b = g["load_w"](("lr", l))
        for tb in range(NTB):
            tok = slice(tb * 512, (tb + 1) * 512)

            def mml():
                ins = None
                for kc in range(8):
                    ins = nc.tensor.matmul(PS[2][0:16, :], wl[:, kc * 16:(kc + 1) * 16], hT[:, kc, tok],
                                           start=(kc == 0), stop=(kc == 7))
                return ins
            P.op("pe", mml, [wl_b, hT_b[tb]], [PS_b[2]])
            P.op("act", lambda: nc.scalar.copy(lrT[:, tok], PS[2][0:16, :]), [PS_b[2]], [lr_b])
        for hd in range(4):
            w, w_b = g["load_w"](("gla1", l, hd))
            wz, wz_b = g["load_w"](("glaz", l, hd))
            P.op("pool", lambda: nc.gpsimd.memset(St[:], 0.0), [], [S_b])
            P.op("pool", lambda: nc.gpsimd.memset(Sb[:], 0.0), [], [Sb_b])
            for tb in range(NTB):
                tok = slice(tb * 512, (tb + 1) * 512)
                for which in range(2):
                    def mm():
                        ins = None
                        for kc in range(8):
                            ins = nc.tensor.matmul(PS[which][:], w[:, kc * 512 + which * 128: kc * 512 + which * 128 + 128],
                                                   hT[:, kc, tok], start=(kc == 0), stop=(kc == 7))
                        return ins
                    P.op("pe", mm, [w_b, hT_b[tb]], [PS_b[which]])
                def mmx():
                    ins = None
                    for t in range(4):
                        tt = slice(tb * 512 + t * 128, tb * 512 + (t + 1) * 128)
                        nc.tensor.matmul(PS[2][:, t * 128:(t + 1) * 128], lrT[:, tt], w2[:, hd * 128:(hd + 1) * 128],
                                         start=True, stop=False)
                        ins = nc.tensor.matmul(PS[2][:, t * 128:(t + 1) * 128], onesb[0:1, :],
                                               glab[0:1, hd * 128:(hd + 1) * 128], start=False, stop=True)
                    return ins
                P.op("pe", mmx, [lr_b, w2_b, cb_b], [PS_b[2]])
                P.op("act", lambda: nc.scalar.activation(out=ex[:], in_=PS[2][:], func=AF.Exp, scale=-1.0),
                     [PS_b[2]], [ex_b])
                P.op("act", lambda: nc.scalar.activation(out=ex[:], in_=ex[:], func=AF.Ln, bias=1.0), [ex_b], [ex_b])
                P.op("dve", lambda: nc.vector.tensor_scalar(la[:], ex[:], -1.0 / 16.0, None, ALU.mult), [ex_b], [la_b])

                def mmg():
                    ins = None
                    for t in range(4):
                        ins = nc.tensor.matmul(PS[3][:, t * 128:(t + 1) * 128], la[:, t * 128:(t + 1) * 128],
                                               C("utincl"), start=True, stop=True)
                    return ins
                P.op("pe", mmg, [la_b, cst_b], [PS_b[3]])

                def mmt():
                    ins = None
                    for t in range(4):
                        ins = nc.tensor.matmul(PS[2][:, t * 128:(t + 1) * 128], C("ltstrict"),
                                               la[:, t * 128:(t + 1) * 128], start=True, stop=True)
                    return ins
                P.op("pe", mmt, [la_b, cst_b], [PS_b[2]])
                P.op("act", lambda: nc.scalar.activation(out=eg[:], in_=PS[3][:], func=AF.Exp), [PS_b[3]], [eg_b])
                P.op("act", lambda: nc.scalar.activation(out=eng[:], in_=PS[3][:], func=AF.Exp, scale=-1.0),
                     [PS_b[3]], [eg_b])
                P.op("act", lambda: nc.scalar.activation(out=edk[:], in_=PS[2][:], func=AF.Exp), [PS_b[2]], [edk_b])
                P.op("dve", lambda: nc.vector.scalar_tensor_tensor(qg[:], PS[0][:], 128.0 ** -0.5, eg[:], ALU.mult, ALU.mult),
                     [PS_b[0], eg_b], [qk_b])
                P.op("dve", lambda: nc.vector.tensor_tensor(kg[:], PS[1][:], eng[:], ALU.mult), [PS_b[1], eg_b], [qk_b])
                P.op("act", lambda: nc.scalar.copy(kb16[:], PS[1][:]), [PS_b[1]], [qk_b])
                p4 = PS[4][:].bitcast(BF16)

                def mmtr():
                    ins = None
                    for t in range(4):
                        ins = nc.tensor.transpose(p4[:, t * 128:(t + 1) * 128], kb16[:, t * 128:(t + 1) * 128], identb[:])
                    return ins
                P.op("pe", mmtr, [qk_b, cb_b], [PS_b[4]])
                P.op("dve", lambda: nc.vector.tensor_tensor(kd[:].rearrange("p a c -> p (a c)"), p4[:, 0:512], edk[:], ALU.mult),
                     [PS_b[4], edk_b], [kd_b])

                def mma():
                    ins = None
                    for t in range(4):
                        ins = nc.tensor.matmul(PS[5][:, t * 128:(t + 1) * 128], kg[:, t * 128:(t + 1) * 128],
                                               qg[:, t * 128:(t + 1) * 128], start=True, stop=True)
                    return ins
                P.op("pe", mma, [qk_b], [PS_b[5]])
                P.op("dve", lambda: nc.vector.tensor_tensor(att[:].rearrange("p a c -> p (a c)"), PS[5][:], C("m01T_incl4"),
                                                            ALU.mult), [PS_b[5], cst_b], [att_b])
                for half in range(2):
                    def mmv():
                        ins = None
                        for tt in range(2):
                            t = half * 2 + tt
                            ts_ = slice(tb * 512 + t * 128, tb * 512 + (t + 1) * 128)
                            for kc in range(8):
                                ins = nc.tensor.matmul(PS[6][:, tt * 256:(tt + 1) * 256], hT[:, kc, ts_],
                                                       w[:, kc * 512 + 256: kc * 512 + 512], start=(kc == 0), stop=(kc == 7))
                        return ins
                    P.op("pe", mmv, [w_b, hT_b[tb]], [PS_b[6]])
                    P.op("act", lambda: nc.scalar.copy(v[:, half * 2:half * 2 + 2, :].rearrange("p a c -> p (a c)"), PS[6][:]),
                         [PS_b[6]], [v_b])
                for vb in range(2):
                    def mmz():
                        ins = None
                        for kc in range(8):
                            ins = nc.tensor.matmul(PS[6][:], wz[:, kc * 256 + vb * 128: kc * 256 + vb * 128 + 128],
                                                   hT[:, kc, tok], start=(kc == 0), stop=(kc == 7))
                        return ins
                    P.op("pe", mmz, [wz_b, hT_b[tb]], [PS_b[6]])
                    P.op("act", lambda: nc.scalar.activation(out=zg[:, vb, :], in_=PS[6][:], func=AF.Silu), [PS_b[6]], [zg_b])
                for t in range(4):
                    for c in range(2):
                        rng = slice(c * 64, (c + 1) * 64)
                        col = slice(t * 128 + c * 64, t * 128 + (c + 1) * 64)

                        def mmo():
                            ins = None
                            for vb in range(2):
                                nc.tensor.matmul(PS[vb][:, col], Sb[:, vb * 128:(vb + 1) * 128], qg[:, col],
                                                 start=True, stop=False)
                                ins = nc.tensor.matmul(PS[vb][:, col], v[rng, t, vb * 128:(vb + 1) * 128],
                                                       att[rng, t, c * 64:(c + 1) * 64], start=False, stop=True)
                            return ins
                        P.op("pe", mmo, [Sb_b, qk_b, v_b, att_b], [PS_b[0], PS_b[1]])
                        P.op("pe", lambda: nc.tensor.matmul(PS[7][:, 0:256], kd[rng, t, :], v[rng, t, :], start=True, stop=True),
                             [kd_b, v_b], [PS_b[7]])
                        last = t * 128 + c * 64 + 63
                        P.op("dve", lambda: nc.vector.scalar_tensor_tensor(St[:], St[:], eg[:, last:last + 1], PS[7][:, 0:256],
                                                                           ALU.mult, ALU.add), [S_b, eg_b, PS_b[7]], [S_b])
                        P.op("act", lambda: nc.scalar.copy(Sb[:], St[:]), [S_b], [Sb_b])
                for vb in range(2):
                    P.op("act", lambda: nc.scalar.activation(out=sq[:], in_=PS[vb][:], func=AF.Square), [PS_b[vb]], [sq_b])
                    P.op("pe", lambda: nc.tensor.matmul(PS[3][:], C("ones"), sq[:], start=(vb == 0), stop=(vb == 1)),
                         [sq_b, cst_b], [PS_b[3]])
                P.op("act", lambda: nc.scalar.activation(out=srt[:], in_=PS[3][:], func=AF.Sqrt, bias=1e-6, scale=1.0 / 256),
                     [PS_b[3]], [srt_b])
                P.op("dve", lambda: nc.vector.reciprocal(rr[:], srt[:]), [srt_b], [rr_b])
                for vb in range(2):
                    P.op("dve", lambda: nc.vector.scalar_tensor_tensor(tmp[:], PS[vb][:], SPc("lon", l * 2 + vb), rr[:],
                                                                       ALU.mult, ALU.mult), [PS_b[vb], rr_b, spar_b], [tmp_b])
                    P.op("pool", lambda: nc.gpsimd.tensor_tensor(ygla[:, hd * 2 + vb, tok], tmp[:], zg[:, vb, :], ALU.mult),
                         [tmp_b, zg_b], [ygla_b[hd]])
        P.barrier()


GSTOP = [0]


class _Stop(Exception):
    pass


def _stage(k):
    if GSTOP[0] == k:
        raise _Stop()


def mixer_gdn(ctx, l, b):
    with contextlib.ExitStack() as ph:
        try:
            _mixer_gdn(ctx, l, b, ph)
        except _Stop:
            ctx["P"].barrier()


def _mixer_gdn(ctx, l, b, ph):
    g = ctx
    nc, P, sb, C, SPc, PS, PS_b = g["nc"], g["P"], g["sb"], g["C"], g["SPc"], g["PS"], g["PS_b"]
    hT, hT_b, S, NTB, NT, L = g["hT"], g["hT_b"], g["S"], g["NTB"], g["NT"], g["L"]
    ygdn, ygdn_b = g["ygdn"], g["ygdn_b"]
    cst_b, cb_b, spar_b, derv_b = g["cst_b"], g["cb_b"], g["spar_b"], g["derv_b"]
    identb, ident4b, dervp = g["identb"], g["ident4b"], g["dervp"]
    NCH = S // 64
    if True:
        def T(name, shape, dt=F32):
            return sb("d" + name, shape, dt, ph), Buf("d" + name)
        w_ba, wba_b = g["load_w"](("ba", l))
        e_t, e_tb = T("e_t", [128, NT * 4])
        lnb_t, lnb_tb = T("lnb_t", [128, NT * 4])
        g_t, g_tb = T("g_t", [128, NT * 4])
        gc_t, gc_tb = T("gc_t", [128, NT * 4])
        g_hm, g_hmb = T("g_hm", [128, 4 * NT])
        lnb_hm, lnb_hmb = T("lnb_hm", [128, 4 * NT])
        beta_hm, beta_hmb = T("beta_hm", [128, 4 * NT])
        bek_hm, bek_hmb = T("bek_hm", [128, 4 * NT])
        e_c, e_cb = T("e_c", [64, NCH * 4])
        lnb_c, lnb_cb = T("lnb_c", [64, NCH * 4])
        g_c, g_cb = T("g_c", [64, NCH * 4])
        x_c, x_cb = T("x_c", [64, NCH * 4])
        g_chm, g_chmb = T("g_chm", [64, 4 * NCH])
        kdec_chm, kdec_chmb = T("kdec_chm", [64, 4 * NCH])
        dtb = SPc("dtb", l * 4, 4)
        nexpA = dervp[:, L + l * 4: L + l * 4 + 4]

        def gates(np_, n, ps, psb, e, eb, lnb, lnbb, gg, ggb, tokfn):
            def mm():
                ins = None
                for i in range(n):
                    for kc in range(8):
                        ins = nc.tensor.matmul(ps[0:np_, i * 8:(i + 1) * 8], hT[:, kc, tokfn(i)], w_ba[:, kc * 8:(kc + 1) * 8],
                                               start=(kc == 0), stop=(kc == 7))
                return ins
            P.op("pe", mm, [wba_b] + hT_b, [psb])
            pv = ps[0:np_, 0:n * 8].rearrange("p (t e) -> p t e", e=8)
            e3 = e[:].rearrange("p (t h) -> p t h", h=4)
            l3 = lnb[:].rearrange("p (t h) -> p t h", h=4)
            g3 = gg[:].rearrange("p (t h) -> p t h", h=4)
            P.op("act", lambda: nc.scalar.activation(out=e3, in_=pv[:, :, 0:4], func=AF.Exp, scale=-1.0), [psb], [eb])
            P.op("act", lambda: nc.scalar.activation(out=lnb[:], in_=e[:], func=AF.Ln, bias=1.0), [eb], [lnbb])
            P.op("dve", lambda: nc.vector.tensor_scalar(lnb[:], lnb[:], -1.0, None, ALU.mult), [lnbb], [lnbb])
            P.op("dve", lambda: nc.vector.tensor_tensor(e3, pv[:, :, 4:8], dtb[0:np_, :].unsqueeze(1).to_broadcast([np_, n, 4]),
                                                        ALU.add), [psb, spar_b, lnbb], [eb])
            P.op("act", lambda: nc.scalar.activation(out=e[:], in_=e[:], func=AF.Exp), [eb], [eb])
            P.op("act", lambda: nc.scalar.activation(out=e[:], in_=e[:], func=AF.Ln, bias=1.0), [eb], [eb])
            P.op("dve", lambda: nc.vector.tensor_tensor(g3, e3, nexpA[0:np_, :].unsqueeze(1).to_broadcast([np_, n, 4]), ALU.mult),
                 [eb, derv_b], [ggb])

        gates(128, NT, PS[0], PS_b[0], e_t, e_tb, lnb_t, lnb_tb, g_t, g_tb, lambda i: slice(i * 128, (i + 1) * 128))
        gates(64, NCH, PS[1], PS_b[1], e_c, e_cb, lnb_c, lnb_cb, g_c, g_cb, lambda i: slice(i * 64, (i + 1) * 64))

        def hm(dst, src, n):
            return dst.rearrange("p (h t) -> p t h", h=4), src.rearrange("p (t h) -> p t h", h=4)
        def mmgc():
            ins = None
            for t in range(NT):
                ins = nc.tensor.matmul(PS[0][:, t * 4:(t + 1) * 4], C("utincl"), g_t[:, t * 4:(t + 1) * 4], start=True, stop=True)
            return ins
        P.op("pe", mmgc, [g_tb, cst_b], [PS_b[0]])
        P.op("dve", lambda: nc.vector.tensor_tensor(gc_t[:], PS[0][:, 0:NT * 4], lnb_t[:], ALU.add), [PS_b[0], lnb_tb], [gc_tb])
        d_, s_ = hm(bek_hm[:], gc_t[:], NT)
        P.op("act", lambda: nc.scalar.activation(out=d_, in_=s_, func=AF.Exp), [gc_tb], [bek_hmb])
        d_, s_ = hm(beta_hm[:], lnb_t[:], NT)
        P.op("act", lambda: nc.scalar.activation(out=d_, in_=s_, func=AF.Exp), [lnb_tb], [beta_hmb])
        d_, s_ = hm(g_hm[:], g_t[:], NT)
        P.op("dve", lambda: nc.vector.tensor_copy(d_, s_), [g_tb], [g_hmb])
        d_, s_ = hm(lnb_hm[:], lnb_t[:], NT)
        P.op("dve", lambda: nc.vector.tensor_copy(d_, s_), [lnb_tb], [lnb_hmb])
        def mmtc():
            ins = None
            for c in range(NCH):
                ins = nc.tensor.matmul(PS[1][0:64, c * 4:(c + 1) * 4], C("ltstrict")[0:64, 0:64], g_c[:, c * 4:(c + 1) * 4],
                                       start=True, stop=True)
            return ins
        P.op("pe", mmtc, [g_cb, cst_b], [PS_b[1]])
        d_ = kdec_chm[:].rearrange("p (h t) -> p t h", h=4)
        P.op("act", lambda: nc.scalar.activation(out=d_, in_=PS[1][0:64, 0:NCH * 4].rearrange("p (t h) -> p t h", h=4),
                                                 func=AF.Exp), [PS_b[1]], [kdec_chmb])
        d_, s_ = hm(g_chm[:], g_c[:], NCH)
        P.op("dve", lambda: nc.vector.tensor_copy(d_, s_), [g_cb], [g_chmb])

        _stage(1)
        xc = [T("xc%d" % i, [128, 515]) for i in range(3)]
        acc, acc_b = T("acc", [128, 512])
        sil, sil_b = T("sil", [128, 512])
        sq, sq_b = T("sq", [128, 512])
        srt, srt_b = T("srt", [128, 512])
        rr, rr_b = T("rr", [128, 512])
        qT, q_b = T("qT", [128, 512], BF16)
        kT, k_b = T("kT", [128, 512], BF16)
        vT, v_b = T("vT", [128, 512], BF16)
        zg, zg_b = T("zg", [128, 512], BF16)
        Gb, Gb_b = T("Gb", [128, 512])
        Gbc, Gbc_b = T("Gbc", [64, 512])
        Dm, Dm_b = T("Dm", [128, 512])
        Ebs, Ebs_b = T("Ebs", [128, 512])
        ET, ET_b = T("ET", [64, 512])
        egr, egr_b = T("egr", [128, 512])
        qd, qd_b = T("qd", [128, 512], BF16)
        Ap = [T("Ap%d" % i, [128, 512], BF16) for i in range(2)]
        Bp = [T("Bp%d" % i, [128, 512], BF16) for i in range(2)]
        TT, TT_b = T("TT", [128, 512], BF16)
        Kbg, Kbg_b = T("Kbg", [128, 4, 128], BF16)
        Vb, Vb_b = T("Vb", [128, 4, 128], BF16)
        Kdc, Kdc_b = T("Kdc", [64, 8, 128], BF16)
        qkT, qkT_b = T("qkT", [64, 512], BF16)
        ub, ub_b = T("ub", [64, 8, 128])
        wT, wT_b = T("wT", [128, 512], BF16)
        ubf, ubf_b = T("ubf", [64, 128], BF16)
        St, S_b = T("S", [128, 128])
        Sb, Sb_b = T("Sb", [128, 128], BF16)
        tmp, tmp_b = T("tmp", [128, 512])
        p4 = PS[4][:].bitcast(BF16)
        for hd in range(4):
            w, w_b = g["load_w"](("gdn", l, hd))
            P.op("pool", lambda: nc.gpsimd.memset(St[:], 0.0), [], [S_b])
            P.op("pool", lambda: nc.gpsimd.memset(Sb[:], 0.0), [], [Sb_b])
            for i in range(3):
                P.op("pool", lambda: nc.gpsimd.memset(xc[i][0][:, 0:3], 0.0), [], [xc[i][1]])
            for tb in range(NTB):
                tok = slice(tb * 512, (tb + 1) * 512)
                acc_s = [(acc, acc_b), (Gb, Gb_b), (Dm, Dm_b)]
                sil_s = [(sil, sil_b), (Ebs, Ebs_b)]
                sq_s = [(sq, sq_b), (tmp, tmp_b)]
                srt_s = [(srt, srt_b), (rr, rr_b)]
                pq = [0, 1, 2]
                for which in range(3):
                    def mm():
                        ins = None
                        for kc in range(8):
                            ins = nc.tensor.matmul(PS[pq[which]][:], w[:, kc * 512 + which * 128: kc * 512 + which * 128 + 128],
                                                   hT[:, kc, tok], start=(kc == 0), stop=(kc == 7))
                        return ins
                    P.op("pe", mm, [w_b, hT_b[tb]], [PS_b[pq[which]]])
                for which in range(3):
                    xcw, xcw_b = xc[which]
                    if tb > 0:
                        P.op("pool", lambda: nc.gpsimd.tensor_copy(xcw[:, 0:3], xcw[:, 512:515]), [xcw_b], [xcw_b])
                    P.op("act", lambda: nc.scalar.copy(xcw[:, 3:515], PS[pq[which]][:]), [PS_b[pq[which]]], [xcw_b])
                cw = lambda which, tap: SPc("conv", l * 48 + (which * 4 + hd) * 4 + tap)
                for which in range(3):
                    xcw, xcw_b = xc[which]
                    a_, a_b = acc_s[which]
                    P.op("act", lambda: nc.scalar.activation(out=a_[:], in_=xcw[:, 0:512], func=AF.Identity, scale=cw(which, 0)),
                         [xcw_b, spar_b], [a_b])
                for tap in range(1, 4):
                    for which in range(3):
                        xcw, xcw_b = xc[which]
                        a_, a_b = acc_s[which]
                        P.op("dve", lambda: nc.vector.scalar_tensor_tensor(a_[:], xcw[:, tap:tap + 512], cw(which, tap), a_[:],
                                                                           ALU.mult, ALU.add), [xcw_b, spar_b, a_b], [a_b])
                for which in range(3):
                    a_, a_b = acc_s[which]
                    if which == 2:
                        P.op("act", lambda: nc.scalar.activation(out=vT[:], in_=a_[:], func=AF.Silu), [a_b], [v_b])
                    else:
                        s_, s_b = sil_s[which]
                        P.op("act", lambda: nc.scalar.activation(out=s_[:], in_=a_[:], func=AF.Silu), [a_b], [s_b])
                for which in range(2):
                    s_, s_b = sil_s[which]
                    q_, q_b2 = sq_s[which]
                    P.op("act", lambda: nc.scalar.activation(out=q_[:], in_=s_[:], func=AF.Square), [s_b], [q_b2])
                stp = [3, 5]
                for which in range(2):
                    q_, q_b2 = sq_s[which]
                    P.op("pe", lambda: nc.tensor.matmul(PS[stp[which]][:], C("ones"), q_[:], start=True, stop=True),
                         [q_b2, cst_b], [PS_b[stp[which]]])
                for which in range(2):
                    r_, r_b = srt_s[which]
                    P.op("act", lambda: nc.scalar.activation(out=r_[:], in_=PS[stp[which]][:], func=AF.Sqrt, bias=1e-6),
                         [PS_b[stp[which]]], [r_b])
                for which in range(2):
                    r_, r_b = srt_s[which]
                    P.op("dve", lambda: nc.vector.reciprocal(r_[:], r_[:]), [r_b], [r_b])
                for which in range(2):
                    s_, s_b = sil_s[which]
                    r_, r_b = srt_s[which]
                    dst, dst_b = (qT, q_b) if which == 0 else (kT, k_b)
                    sc = 128.0 ** -0.5 if which == 0 else 1.0
                    P.op("dve", lambda: nc.vector.scalar_tensor_tensor(dst[:], s_[:], sc, r_[:], ALU.mult, ALU.mult),
                         [s_b, r_b], [dst_b])
                _stage(2)
                def mmz():
                    ins = None
                    for kc in range(8):
                        ins = nc.tensor.matmul(PS[0][:], w[:, kc * 512 + 384: kc * 512 + 512], hT[:, kc, tok],
                                               start=(kc == 0), stop=(kc == 7))
                    return ins
                P.op("pe", mmz, [w_b, hT_b[tb]], [PS_b[0]])
                P.op("act", lambda: nc.scalar.activation(out=zg[:], in_=PS[0][:], func=AF.Silu), [PS_b[0]], [zg_b])
                gsl = g_hm[:, hd * NT + tb * 4: hd * NT + tb * 4 + 4]
                P.op("dve", lambda: nc.vector.tensor_copy(Gb[:].rearrange("p (t c) -> p t c", c=128),
                                                          gsl.unsqueeze(2).to_broadcast([128, 4, 128])), [g_hmb], [Gb_b])
                gcs = g_chm[:, hd * NCH + tb * 8: hd * NCH + tb * 8 + 8]
                P.op("dve", lambda: nc.vector.tensor_copy(Gbc[:].rearrange("p (t c) -> p t c", c=64),
                                                          gcs.unsqueeze(2).to_broadcast([64, 8, 64])), [g_chmb], [Gbc_b])

                def mmD():
                    ins = None
                    for t in range(4):
                        cs = slice(t * 128, (t + 1) * 128)
                        nc.tensor.matmul(PS[2][:, cs], C("utincl"), Gb[:, cs], start=True, stop=False)
                        ins = nc.tensor.matmul(PS[2][:, cs], Gb[:, cs], C("nutincl"), start=False, stop=True)
                    return ins
                P.op("pe", mmD, [Gb_b, cst_b], [PS_b[2]])
                P.op("dve", lambda: nc.vector.tensor_tensor(Dm[:], PS[2][:], C("negm_strict4"), ALU.add), [PS_b[2], cst_b], [Dm_b])
                for t in range(4):
                    cs = slice(t * 128, (t + 1) * 128)
                    bi = hd * NT + tb * 4 + t
                    P.op("act", lambda: nc.scalar.activation(out=Ebs[:, cs], in_=Dm[:, cs], func=AF.Exp, bias=lnb_hm[:, bi:bi + 1]),
                         [Dm_b, lnb_hmb], [Ebs_b])
                _stage(3)
                def mmG():
                    ins = None
                    for t in range(4):
                        cs = slice(t * 128, (t + 1) * 128)
                        ins = nc.tensor.matmul(PS[3][:, cs], Gb[:, cs], C("utincl"), start=True, stop=True)
                    return ins
                P.op("pe", mmG, [Gb_b, cst_b], [PS_b[3]])
                P.op("act", lambda: nc.scalar.activation(out=egr[:], in_=PS[3][:], func=AF.Exp), [PS_b[3]], [egr_b])
                P.op("pool", lambda: nc.gpsimd.tensor_tensor(qd[:], qT[:], egr[:], ALU.mult), [q_b, egr_b], [qd_b])
                _stage(4)
                def mmM():
                    ins = None
                    for t in range(4):
                        cs = slice(t * 128, (t + 1) * 128)
                        ins = nc.tensor.matmul(PS[2][:, cs], kT[:, cs], kT[:, cs], start=True, stop=True)
                    return ins
                P.op("pe", mmM, [k_b], [PS_b[2]])
                A0, A0_b = Ap[0]
                P.op("dve", lambda: nc.vector.tensor_tensor(A0[:], PS[2][:], Ebs[:], ALU.mult), [PS_b[2], Ebs_b], [A0_b])

                def mmB():
                    ins = None
                    for t in range(4):
                        cs = slice(t * 128, (t + 1) * 128)
                        ins = nc.tensor.transpose(p4[:, cs], A0[:, cs], identb[:])
                    return ins
                P.op("pe", mmB, [A0_b, cb_b], [PS_b[4]])
                B0, B0_b = Bp[0]
                P.op("dve", lambda: nc.vector.scalar_tensor_tensor(TT[:], p4[:, 0:512], -1.0, ident4b[:], ALU.mult, ALU.add),
                     [PS_b[4], cb_b], [TT_b])
                P.op("dve", lambda: nc.vector.tensor_copy(B0[:], p4[:, 0:512]), [PS_b[4]], [B0_b])
                _stage(5)
                cur = 0
                for lev in range(5):
                    Ac, Ac_b = Ap[cur]
                    Bc, Bc_b = Bp[cur]
                    An, An_b = Ap[1 - cur]
                    Bn, Bn_b = Bp[1 - cur]

                    def mmA2():
                        ins = None
                        for t in range(4):
                            cs = slice(t * 128, (t + 1) * 128)
                            ins = nc.tensor.matmul(PS[2][:, cs], Bc[:, cs], Ac[:, cs], start=True, stop=True)
                        return ins
                    P.op("pe", mmA2, [Ac_b, Bc_b], [PS_b[2]])
                    P.op("act", lambda: nc.scalar.copy(An[:], PS[2][:]), [PS_b[2]], [An_b])
                    if lev < 4:
                        def mmB2():
                            ins = None
                            for t in range(4):
                                cs = slice(t * 128, (t + 1) * 128)
                                ins = nc.tensor.matmul(PS[3][:, cs], Ac[:, cs], Bc[:, cs], start=True, stop=True)
                            return ins
                        P.op("pe", mmB2, [Ac_b, Bc_b], [PS_b[3]])
                        P.op("dve", lambda: nc.vector.tensor_copy(Bn[:], PS[3][:]), [PS_b[3]], [Bn_b])

                    def mmT():
                        ins = None
                        for t in range(4):
                            cs = slice(t * 128, (t + 1) * 128)
                            ins = nc.tensor.matmul(PS[5][:, cs], An[:, cs], TT[:, cs], start=True, stop=True)
                        return ins
                    P.op("pe", mmT, [An_b, TT_b], [PS_b[5]])
                    P.op("dve", lambda: nc.vector.tensor_tensor(TT[:], TT[:], PS[5][:], ALU.add), [PS_b[5], TT_b], [TT_b])
                    cur = 1 - cur
                _stage(6)
                def mmKt():
                    ins = None
                    for t in range(4):
                        cs = slice(t * 128, (t + 1) * 128)
                        ins = nc.tensor.transpose(p4[:, cs], kT[:, cs], identb[:])
                    return ins
                P.op("pe", mmKt, [k_b, cb_b], [PS_b[4]])
                ci = hd * NT + tb * 4
                P.op("dve", lambda: nc.vector.tensor_tensor(Kbg[:], p4[:, 0:512].rearrange("p (t c) -> p t c", c=128),
                                                            bek_hm[:, ci:ci + 4].unsqueeze(2).to_broadcast([128, 4, 128]), ALU.mult),
                     [PS_b[4], bek_hmb], [Kbg_b])

                def mmVt():
                    ins = None
                    for t in range(4):
                        cs = slice(t * 128, (t + 1) * 128)
                        ins = nc.tensor.transpose(p4[:, cs], vT[:, cs], identb[:])
                    return ins
                P.op("pe", mmVt, [v_b, cb_b], [PS_b[4]])
                P.op("dve", lambda: nc.vector.tensor_tensor(Vb[:], p4[:, 0:512].rearrange("p (t c) -> p t c", c=128),
                                                            beta_hm[:, ci:ci + 4].unsqueeze(2).to_broadcast([128, 4, 128]), ALU.mult),
                     [PS_b[4], beta_hmb], [Vb_b])
                _stage(7)
                def mmKc():
                    ins = None
                    for ch in range(8):
                        ins = nc.tensor.transpose(p4[0:64, ch * 128:(ch + 1) * 128], kT[:, ch * 64:(ch + 1) * 64], identb[:])
                    return ins
                P.op("pe", mmKc, [k_b, cb_b], [PS_b[4]])
                cj = hd * NCH + tb * 8
                P.op("dve", lambda: nc.vector.tensor_tensor(Kdc[:], p4[0:64, :].rearrange("p (t c) -> p t c", c=128),
                                                            kdec_chm[:, cj:cj + 8].unsqueeze(2).to_broadcast([64, 8, 128]), ALU.mult),
                     [PS_b[4], kdec_chmb], [Kdc_b])
                _stage(8)
                def mmDc():
                    ins = None
                    for ch in range(8):
                        cs = slice(ch * 64, (ch + 1) * 64)
                        nc.tensor.matmul(PS[5][0:64, cs], C("utincl")[0:64, 0:64], Gbc[:, cs], start=True, stop=False)
                        ins = nc.tensor.matmul(PS[5][0:64, cs], Gbc[:, cs], C("nutincl")[0:64, 0:64], start=False, stop=True)
                    return ins
                P.op("pe", mmDc, [Gbc_b, cst_b], [PS_b[5]])
                negT = C("negmT_incl4")[0:64, 0:64].unsqueeze(1).to_broadcast([64, 8, 64])
                P.op("dve", lambda: nc.vector.tensor_tensor(ET[:].rearrange("p (t c) -> p t c", c=64), negT,
                                                            PS[5][0:64, :].rearrange("p (t c) -> p t c", c=64), ALU.subtract),
                     [PS_b[5], cst_b], [ET_b])
                P.op("act", lambda: nc.scalar.activation(out=ET[:], in_=ET[:], func=AF.Exp), [ET_b], [ET_b])

                def mmQK():
                    ins = None
                    for ch in range(8):
                        cs = slice(ch * 64, (ch + 1) * 64)
                        ins = nc.tensor.matmul(PS[3][0:64, cs], kT[:, cs], qT[:, cs], start=True, stop=True)
                    return ins
                P.op("pe", mmQK, [k_b, q_b], [PS_b[3]])
                P.op("dve", lambda: nc.vector.tensor_tensor(qkT[:], PS[3][0:64, :], ET[:], ALU.mult), [PS_b[3], ET_b], [qkT_b])
                _stage(9)
                for half in range(2):
                    def mmU():
                        ins = None
                        for k4 in range(4):
                            ch = half * 4 + k4
                            t, c = ch // 2, ch % 2
                            ins = nc.tensor.matmul(PS[6 + half][0:64, k4 * 128:(k4 + 1) * 128],
                                                   TT[:, t * 128 + c * 64: t * 128 + (c + 1) * 64], Vb[:, t, :], start=True, stop=True)
                        return ins
                    P.op("pe", mmU, [TT_b, Vb_b], [PS_b[6 + half]])
                    P.op("act", lambda: nc.scalar.copy(ub[:, half * 4:(half + 1) * 4, :].rearrange("p a c -> p (a c)"),
                                                       PS[6 + half][0:64, :]), [PS_b[6 + half]], [ub_b])

                def mmW():
                    ins = None
                    for ch in range(8):
                        t, c = ch // 2, ch % 2
                        ins = nc.tensor.matmul(PS[5][:, ch * 64:(ch + 1) * 64], Kbg[:, t, :],
                                               TT[:, t * 128 + c * 64: t * 128 + (c + 1) * 64], start=True, stop=True)
                    return ins
                P.op("pe", mmW, [Kbg_b, TT_b], [PS_b[5]])
                P.op("dve", lambda: nc.vector.tensor_copy(wT[:], PS[5][:]), [PS_b[5]], [wT_b])
                _stage(10)
                for ch in range(8):
                    cs = slice(ch * 64, (ch + 1) * 64)
                    P.op("pe", lambda: nc.tensor.matmul(PS[6][0:64, 0:128], wT[:, cs], Sb[:], start=True, stop=True),
                         [wT_b, Sb_b], [PS_b[6]])
                    P.op("dve", lambda: nc.vector.tensor_tensor(ubf[:], ub[:, ch, :], PS[6][0:64, 0:128], ALU.subtract),
                         [ub_b, PS_b[6]], [ubf_b])

                    def mmO():
                        nc.tensor.matmul(PS[1][:, cs], Sb[:], qd[:, cs], start=True, stop=False)
                        return nc.tensor.matmul(PS[1][:, cs], ubf[:], qkT[:, cs], start=False, stop=True)
                    P.op("pe", mmO, [Sb_b, qd_b, ubf_b, qkT_b], [PS_b[1]])
                    P.op("pe", lambda: nc.tensor.matmul(PS[7][:, 0:128], Kdc[:, ch, :], ubf[:], start=True, stop=True),
                         [Kdc_b, ubf_b], [PS_b[7]])
                    last = ch * 64 + 63
                    P.op("dve", lambda: nc.vector.scalar_tensor_tensor(St[:], St[:], egr[:, last:last + 1], PS[7][:, 0:128],
                                                                       ALU.mult, ALU.add), [S_b, egr_b, PS_b[7]], [S_b])
                    P.op("act", lambda: nc.scalar.copy(Sb[:], St[:]), [S_b], [Sb_b])
                _stage(11)
                P.op("act", lambda: nc.scalar.activation(out=sq[:], in_=PS[1][:], func=AF.Square), [PS_b[1]], [sq_b])
                P.op("pe", lambda: nc.tensor.matmul(PS[0][:], C("ones"), sq[:], start=True, stop=True), [sq_b, cst_b], [PS_b[0]])
                P.op("act", lambda: nc.scalar.activation(out=srt[:], in_=PS[0][:], func=AF.Sqrt, bias=1e-6, scale=1.0 / 128),
                     [PS_b[0]], [srt_b])
                P.op("dve", lambda: nc.vector.reciprocal(rr[:], srt[:]), [srt_b], [rr_b])
                P.op("dve", lambda: nc.vector.scalar_tensor_tensor(tmp[:], PS[1][:], SPc("gon", l), rr[:], ALU.mult, ALU.mult),
                     [PS_b[1], rr_b, spar_b], [tmp_b])
                P.op("pool", lambda: nc.gpsimd.tensor_tensor(ygdn[:, hd, tok], tmp[:], zg[:], ALU.mult), [tmp_b, zg_b], [ygdn_b[hd]])
        P.barrier()


def mixer_fake(which):
    def f(ctx, l, b):
        g = ctx
        nc, P = g["nc"], g["P"]
        hT, hT_b = g["hT"], g["hT_b"]
        if which == "sb":
            P.op("pool", lambda: nc.gpsimd.tensor_copy(g["ysb"][:], hT[:, 0:4, :]), hT_b, g["ysb_b"])
        elif which == "gdn":
            P.op("pool", lambda: nc.gpsimd.tensor_copy(g["ygdn"][:], hT[:, 4:8, :]), hT_b, g["ygdn_b"])
        else:
            P.op("pool", lambda: nc.gpsimd.tensor_copy(g["ygla"][:], hT[:, :, :]), hT_b, g["ygla_b"])
    return f


MIXERS = [mixer_sb, mixer_gdn, mixer_gla]


def kernel(**inputs):
    NSEQ, S, L, NCORE = 2, 2048, 4, 8
    inp = {k: np.asarray(v) for k, v in inputs.items()}
    common = prep_common(inp, L, NSEQ)
    nc, _ = build_program(NSEQ, S, L)
    x = inp["x"]
    c = inp["c"]
    in_maps = []
    for core in range(NCORE):
        m = dict(common)
        xb = x[core * NSEQ:(core + 1) * NSEQ]
        m["xT"] = np.ascontiguousarray(xb.transpose(0, 2, 1))
        cb = c[core * NSEQ:(core + 1) * NSEQ]
        m["cT"] = np.ascontiguousarray(cb.reshape(NSEQ, 8, 128).transpose(2, 1, 0).reshape(128, 8 * NSEQ))
        in_maps.append(m)
    res = run_bass_kernel_spmd(nc, in_maps, core_ids=list(range(NCORE)))
    outs = [np.asarray(r["outT"]).transpose(0, 2, 1) for r in res.results]
    return np.ascontiguousarray(np.concatenate(outs, axis=0).astype(np.float32))
```

```python
import contextlib
import numpy as np
import concourse.bass as bass
import concourse.mybir as mybir
from concourse.bass_utils import run_bass_kernel_spmd

F32 = mybir.dt.float32
BF16 = mybir.dt.bfloat16
AF = mybir.ActivationFunctionType
ALU = mybir.AluOpType
CH = 30000
NEG = -30000.0

D = 1024
O_SQ, O_SK, O_SV, O_SZ = 0, 512, 1024, 1536
O_DQ, O_DK, O_DV, O_DZ, O_DB, O_DA = 2048, 2560, 3072, 3584, 4096, 4100
O_LQ, O_LK, O_LV, O_LZ, O_LR, O_MG = 4104, 4616, 5128, 6152, 7176, 7192
N_IN = 10264


class Buf:
    __slots__ = ("name", "w", "r")

    def __init__(self, name):
        self.name = name
        self.w = None
        self.r = {}


class Eng:
    def __init__(self, name, h):
        self.name = name
        self.h = h
        self.sems = []
        self.count = 0
        self.known = {}


class Slot:
    def __init__(self, sid, sem):
        self.id = sid
        self.sem = sem
        self.val = 0


class Prog:
    def __init__(self, nc):
        self.nc = nc
        self.E = {n: Eng(n, h) for n, h in (("pe", nc.tensor), ("act", nc.scalar), ("dve", nc.vector),
                                            ("pool", nc.gpsimd), ("sp", nc.sync))}
        self.slots = []
        self.nins = 0

    def slot(self, name):
        s = Slot(len(self.slots), self.nc.alloc_semaphore(name="dq_" + name))
        self.slots.append(s)
        return s

    def _wait(self, E, tok):
        kind, who, val = tok
        if kind == "e" and who == "pe" and E.name == "pe":
            return
        key = (kind, who)
        if E.known.get(key, 0) >= val:
            return
        E.known[key] = val
        if kind == "e":
            F = self.E[who]
            semi, v = divmod(val - 1, CH)
            E.h.wait_ge(F.sems[semi], v + 1)
        else:
            E.h.wait_ge(self.slots[who].sem, val)
        self.nins += 1

    def _deps(self, E, reads, writes, skip=None):
        for b in reads:
            if b.w is not None and (b.w[0], b.w[1]) != skip:
                self._wait(E, b.w)
        for b in writes:
            if b.w is not None and (b.w[0], b.w[1]) != skip:
                self._wait(E, b.w)
            for t in list(b.r.values()):
                if (t[0], t[1]) != skip:
                    self._wait(E, t)

    def op(self, eng, fn, reads=(), writes=()):
        E = self.E[eng]
        self._deps(E, reads, writes)
        ins = fn()
        E.count += 1
        semi, _ = divmod(E.count - 1, CH)
        while len(E.sems) <= semi:
            E.sems.append(self.nc.alloc_semaphore(name="%s_s%d" % (eng, len(E.sems))))
        ins.then_inc(E.sems[semi], 1)
        self.nins += 1
        tok = ("e", eng, E.count)
        for b in reads:
            b.r[("e", eng)] = tok
        for b in writes:
            b.w = tok
            b.r = {}
        return tok

    def dma(self, out_ap, in_ap, reads, writes, slot, queue="sp"):
        E = self.E[queue]
        self._deps(E, reads, writes, skip=("d", slot.id))
        ins = E.h.dma_start(out=out_ap, in_=in_ap)
        slot.val += 16
        assert slot.val < 60000, "dma slot overflow"
        ins.then_inc(slot.sem, 16)
        self.nins += 1
        tok = ("d", slot.id, slot.val)
        for b in reads:
            b.r[("d", slot.id)] = tok
        for b in writes:
            b.w = tok
            b.r = {}
        return tok

    def barrier(self):
        for E in self.E.values():
            for F in self.E.values():
                if F is not E and F.count > 0 and F.name != "sp":
                    self._wait(E, ("e", F.name, F.count))
            for s in self.slots:
                if s.val > 0:
                    self._wait(E, ("d", s.id, s.val))


def host_consts():
    p = np.arange(128)[:, None]
    f = np.arange(128)[None, :]
    same = (p // 64) == (f // 64)
    c = {}
    c["ident"] = (p == f).astype(np.float32)
    c["ones"] = np.ones((128, 128), np.float32)
    c["utincl"] = ((p <= f) & same).astype(np.float32)
    c["nutincl"] = -c["utincl"]
    c["ltstrict"] = ((p > f) & same).astype(np.float32)
    negm_strict = np.where((p > f) & same, 0.0, NEG).astype(np.float32)
    negmT_incl = np.where((f >= p) & same, 0.0, NEG).astype(np.float32)
    m01T_incl = ((f >= p) & same).astype(np.float32)
    c["negm_strict4"] = np.tile(negm_strict, (1, 4))
    c["negmT_incl4"] = np.tile(negmT_incl, (1, 4))
    c["m01T_incl4"] = np.tile(m01T_incl, (1, 4))
    c["ident4"] = np.tile(c["ident"], (1, 4))
    c["nuincl_sb"] = -(p >= f).astype(np.float32)
    f5 = np.arange(512)[None, :]
    sbm = [((p + 128 * m) < f5) for m in range(4)]
    c["sbmask01"] = np.concatenate([m.astype(np.float32) for m in sbm], axis=1)
    c["sbnegmask"] = np.concatenate([np.where(m, 0.0, NEG).astype(np.float32) for m in sbm], axis=1)
    return c


CONST_ORDER = ["ident", "ones", "utincl", "nutincl", "ltstrict", "negm_strict4", "negmT_incl4", "m01T_incl4"]
CONSTB_ORDER = ["nuincl_sb", "ident4", "sbmask01", "sbnegmask"]


def const_layout():
    c = host_consts()
    off = {}
    o = 0
    for k in CONST_ORDER:
        off[k] = (o, c[k].shape[1])
        o += c[k].shape[1]
    arr = np.concatenate([c[k] for k in CONST_ORDER], axis=1).astype(np.float32)
    arrb = np.concatenate([c[k] for k in CONSTB_ORDER], axis=1).astype(np.float32)
    return arr, off, arrb


def blk(w, cols):
    sub = w[:, cols]
    n = sub.shape[1]
    return np.ascontiguousarray(sub.reshape(8, 128, n).transpose(1, 0, 2).reshape(128, 8 * n))


def fm(v):
    return np.ascontiguousarray(v.reshape(-1, 128).T)


def prep_common(inp, L, NSEQ):
    ar = np.arange
    out = {}
    cst, _, cstb = const_layout()
    out["cst"] = cst
    out["cstb"] = cstb
    w_in = inp["w_in"]
    out["ada_w"] = np.stack([blk(inp["ada_w"][l], ar(3072)).reshape(128, 8, 3072) for l in range(L)])
    out["ada_b2"] = np.ascontiguousarray(np.stack(
        [np.repeat(fm(inp["ada_b"][l])[:, :, None], NSEQ, axis=2) for l in range(L)], axis=1))
    out["norm_g2"] = np.ascontiguousarray(np.stack(
        [np.repeat(fm(inp["norm_g"][l])[:, :, None], NSEQ, axis=2) for l in range(L)], axis=1))
    w_sb = np.empty((L, 4, 128, 4096), np.float32)
    w_gdn = np.empty((L, 4, 128, 4096), np.float32)
    w_gla1 = np.empty((L, 4, 128, 4096), np.float32)
    w_glaz = np.empty((L, 4, 128, 2048), np.float32)
    w_ba = np.empty((L, 128, 64), np.float32)
    w_lr = np.empty((L, 128, 128), np.float32)
    w_mg = np.empty((L, 8, 128, 3072), np.float32)
    w_pj = np.empty((L, 8, 128, 2048), np.float32)
    w_o = np.empty((L, 8, 128, 1024), np.float32)
    for l in range(L):
        w = w_in[l]
        for h in range(4):
            a = ar(128) + 128 * h
            w_sb[l, h] = blk(w, np.concatenate([O_SQ + a, O_SK + a, O_SV + a, O_SZ + a]))
            w_gdn[l, h] = blk(w, np.concatenate([O_DQ + a, O_DK + a, O_DV + a, O_DZ + a]))
            a2 = ar(256) + 256 * h
            w_gla1[l, h] = blk(w, np.concatenate([O_LQ + a, O_LK + a, O_LV + a2]))
            w_glaz[l, h] = blk(w, O_LZ + a2)
        w_ba[l] = blk(w, O_DB + ar(8))
        w_lr[l] = blk(w, O_LR + ar(16))
        for nb in range(8):
            a = ar(128) + 128 * nb
            w_mg[l, nb] = blk(w, np.concatenate([O_MG + a, O_MG + 1024 + a, O_MG + 2048 + a]))
            psb = inp["proj_sb"][l][:, a].reshape(4, 128, 128).transpose(1, 0, 2).reshape(128, 512)
            pgd = inp["proj_gdn"][l][:, a].reshape(4, 128, 128).transpose(1, 0, 2).reshape(128, 512)
            pgl = inp["proj_gla"][l][:, a].reshape(8, 128, 128).transpose(1, 0, 2).reshape(128, 1024)
            w_pj[l, nb] = np.concatenate([psb, pgd, pgl], axis=1)
            w_o[l, nb] = inp["w_out"][l][:, a].reshape(8, 128, 128).transpose(1, 0, 2).reshape(128, 1024)
    out.update(w_sb=w_sb, w_gdn=w_gdn, w_gla1=w_gla1, w_glaz=w_glaz, w_ba=w_ba, w_lr=w_lr, w_mg=w_mg,
               w_pj=w_pj, w_o=w_o)
    sp = {}
    sp["qn"] = np.stack([inp["sb_qnorm"][l] for l in range(L)], axis=1)
    sp["kn"] = np.stack([inp["sb_knorm"][l] for l in range(L)], axis=1)
    sp["conv"] = np.stack([inp["gdn_conv"][l].T.reshape(12, 128, 4).transpose(1, 0, 2).reshape(128, 48)
                           for l in range(L)], axis=1).reshape(128, L * 48)
    sp["gon"] = np.stack([inp["gdn_onorm"][l] for l in range(L)], axis=1)
    sp["lon"] = np.stack([fm(inp["gla_onorm"][l]) for l in range(L)], axis=1).reshape(128, L * 2)
    sp["mb"] = np.stack([fm(inp["merge_b"][l]) for l in range(L)], axis=1).reshape(128, L * 24)
    sp["alog"] = np.tile(np.stack([inp["gdn_a_log"][l] for l in range(L)], 0).reshape(1, L * 4), (128, 1))
    sp["dtb"] = np.tile(np.stack([inp["gdn_dt_bias"][l] for l in range(L)], 0).reshape(1, L * 4), (128, 1))
    order = ["qn", "kn", "conv", "gon", "lon", "mb", "alog", "dtb"]
    out["ada_b2"] = out["ada_b2"].reshape(128, -1)
    out["norm_g2"] = out["norm_g2"].reshape(128, -1)
    out["spar"] = np.ascontiguousarray(np.concatenate([sp[k].astype(np.float32) for k in order], axis=1))
    out["gla_w2"] = np.ascontiguousarray(inp["gla_w2"][:L].astype(np.float32))
    out["gla_b"] = np.ascontiguousarray(inp["gla_b"][:L].reshape(L, 1, 512).astype(np.float32))
    return out


def spar_offsets(L):
    sizes = [("qn", L), ("kn", L), ("conv", L * 48), ("gon", L), ("lon", L * 2), ("mb", L * 24),
             ("alog", L * 4), ("dtb", L * 4)]
    off = {}
    o = 0
    for k, n in sizes:
        off[k] = o
        o += n
    return off, o


def build_program(NSEQ, S, L, dbg=()):
    nc = bass.Bass("TRN2", target_bir_lowering=False)
    P = Prog(nc)
    NTB = S // 512
    NT = S // 128
    cst_np, coff, cstb_np = const_layout()
    NCST = cst_np.shape[1]
    spo, NSP = spar_offsets(L)

    def din(name, shape, dt=F32):
        return nc.dram_tensor(name, list(shape), dt, kind="ExternalInput").ap()

    xT_d = din("xT", [NSEQ, D, S])
    cT_d = din("cT", [128, 8 * NSEQ])
    cst_d = din("cst", [128, NCST])
    cstb_d = din("cstb", [128, cstb_np.shape[1]])
    adaw_d = din("ada_w", [L, 128, 8, 3072])
    adab_d = din("ada_b2", [128, L * 24 * NSEQ])
    ng_d = din("norm_g2", [128, L * 8 * NSEQ])
    wsb_d = din("w_sb", [L, 4, 128, 4096])
    wgdn_d = din("w_gdn", [L, 4, 128, 4096])
    wgla1_d = din("w_gla1", [L, 4, 128, 4096])
    wglaz_d = din("w_glaz", [L, 4, 128, 2048])
    wba_d = din("w_ba", [L, 128, 64])
    wlr_d = din("w_lr", [L, 128, 128])
    wmg_d = din("w_mg", [L, 8, 128, 3072])
    wpj_d = din("w_pj", [L, 8, 128, 2048])
    wo_d = din("w_o", [L, 8, 128, 1024])
    spar_d = din("spar", [128, NSP])
    w2_d = din("gla_w2", [L, 16, 512])
    glab_d = din("gla_b", [L, 1, 512])
    outT_d = nc.dram_tensor("outT", [NSEQ, D, S], F32, kind="ExternalOutput").ap()
    xs_d = nc.dram_tensor("xs", [NSEQ, D, S], F32, kind="Internal").ap()
    dbg_d = {}
    for name, shape in dbg:
        dbg_d[name] = nc.dram_tensor("dbg_" + name, list(shape), F32, kind="ExternalOutput").ap()

    es = contextlib.ExitStack()

    uid = [0]

    def sb(name, shape, dt=F32, stack=None):
        uid[0] += 1
        return (stack or es).enter_context(nc.sbuf_tensor("%s_u%d" % (name, uid[0]), list(shape), dt))

    with es:
        cst = sb("cst", [128, NCST])
        cst_b = Buf("cst")
        identb = sb("identb", [128, 128], BF16)
        onesb = sb("onesb", [128, 128], BF16)
        nuinclb = sb("nuinclb", [128, 128], BF16)
        sbnegb = sb("sbnegb", [128, 2048], BF16)
        sbm01b = sb("sbm01b", [128, 2048], BF16)
        ident4b = sb("ident4b", [128, 512], BF16)
        cb_b = Buf("cstb")
        spar = sb("spar", [128, NSP])
        spar_b = Buf("spar")
        cact = sb("cact", [128, 8 * NSEQ])
        cact_b = Buf("cact")
        adab = sb("adab", [128, L * 24 * NSEQ])
        ng2 = sb("ng2", [128, L * 8 * NSEQ])
        modT = sb("modT", [128, L * 24 * NSEQ])
        Amod = sb("Amod", [128, L * 8 * NSEQ])
        mod_b = Buf("mod")
        dervp = sb("dervp", [128, 5 * L + 8])
        derv_b = Buf("derv")
        hT = sb("hT", [128, 8, S], BF16)
        hT_b = [Buf("hT%d" % t) for t in range(NTB)]
        ysb = sb("ysb", [128, 4, S], BF16)
        ygdn = sb("ygdn", [128, 4, S], BF16)
        ygla = sb("ygla", [128, 8, S], BF16)
        ysb_b = [Buf("ysb%d" % h) for h in range(4)]
        ygdn_b = [Buf("ygdn%d" % h) for h in range(4)]
        ygla_b = [Buf("ygla%d" % h) for h in range(4)]
        NRING, PF = 4, 2
        ring = [sb("ring%d" % i, [128, 4096], BF16) for i in range(NRING)]
        ring_b = [Buf("ring%d" % i) for i in range(NRING)]
        ring_s = [P.slot("ring%d" % i) for i in range(NRING)]
        wsched = []
        for b_ in range(NSEQ):
            for l_ in range(L):
                for h_ in range(4):
                    wsched.append((("sb", l_, h_), wsb_d[l_, h_], 4096))
                wsched.append((("ba", l_), wba_d[l_], 64))
                for h_ in range(4):
                    wsched.append((("gdn", l_, h_), wgdn_d[l_, h_], 4096))
                wsched.append((("lr", l_), wlr_d[l_], 128))
                for h_ in range(4):
                    wsched.append((("gla1", l_, h_), wgla1_d[l_, h_], 4096))
                    wsched.append((("glaz", l_, h_), wglaz_d[l_, h_], 2048))
                for n_ in range(8):
                    wsched.append((("mg", l_, n_), wmg_d[l_, n_], 3072))
                    wsched.append((("pj", l_, n_), wpj_d[l_, n_], 2048))
                for n_ in range(8):
                    wsched.append((("wo", l_, n_), wo_d[l_, n_], 1024))
        wstate = {"next": 0, "issued": 0}
        PS = [es.enter_context(nc.psum_tensor("ps%d" % i, [128, 512], F32)) for i in range(8)]
        PS_b = [Buf("ps%d" % i) for i in range(8)]
        misc_s = P.slot("misc")
        p1_slots = [P.slot("p1x%d" % i) for i in range(2)]
        p5_slots = [P.slot("p5x%d" % i) for i in range(2)]
        misc_b = Buf("miscload")

        def C(name, a=0, b=None):
            o, n = coff[name]
            return cst[:, o + a: o + (n if b is None else b)]

        def SPc(name, i, n=1):
            o = spo[name] + i
            return spar[:, o:o + n]

        P.dma(cst[:], cst_d[:, :], [], [cst_b], misc_s)
        P.dma(spar[:], spar_d[:, :], [], [spar_b], misc_s)
        P.dma(cact[:], cT_d[:, :], [], [cact_b], misc_s)
        P.dma(adab[:], adab_d[:, :], [], [misc_b], misc_s)
        P.dma(ng2[:], ng_d[:, :], [], [misc_b], misc_s)
        P.op("dve", lambda: nc.vector.tensor_copy(identb[:], C("ident")), [cst_b], [cb_b])
        P.op("dve", lambda: nc.vector.tensor_copy(onesb[:], C("ones")), [cst_b], [cb_b])
        stg_es = contextlib.ExitStack()
        stage = [sb("stage%d" % i, [128, 4096], F32, stg_es) for i in range(2)]
        stage_b = [Buf("stage%d" % i) for i in range(2)]
        stage_s = [P.slot("stage%d" % i) for i in range(2)]
        P.dma(stage[0][:, 0:2688], cstb_d[:, 0:2688], [], [stage_b[0]], stage_s[0])
        P.op("dve", lambda: nc.vector.tensor_copy(nuinclb[:], stage[0][:, 0:128]), [stage_b[0]], [cb_b])
        P.op("dve", lambda: nc.vector.tensor_copy(ident4b[:], stage[0][:, 128:640]), [stage_b[0]], [cb_b])
        P.op("dve", lambda: nc.vector.tensor_copy(sbm01b[:], stage[0][:, 640:2688]), [stage_b[0]], [cb_b])
        P.dma(stage[1][:, 0:2048], cstb_d[:, 2688:4736], [], [stage_b[1]], stage_s[1])
        P.op("dve", lambda: nc.vector.tensor_copy(sbnegb[:], stage[1][:, 0:2048]), [stage_b[1]], [cb_b])
        P.op("act", lambda: nc.scalar.activation(out=cact[:], in_=cact[:], func=AF.Silu), [cact_b], [cact_b])
        P.op("dve", lambda: nc.vector.tensor_scalar(dervp[:, 0:L], SPc("qn", 0, L), 128.0 ** -0.5, None, ALU.mult),
             [spar_b], [derv_b])

        P.op("act", lambda: nc.scalar.activation(out=dervp[:, L:L + 4 * L], in_=SPc("alog", 0, 4 * L), func=AF.Exp),
             [spar_b, derv_b], [derv_b])
        P.op("dve", lambda: nc.vector.tensor_scalar(dervp[:, L:L + 4 * L], dervp[:, L:L + 4 * L], -1.0, None, ALU.mult),
             [derv_b], [derv_b])

        def load_w(key):
            i = wstate["next"]
            wstate["next"] += 1
            assert wsched[i][0] == key, (wsched[i][0], key)
            while wstate["issued"] < min(len(wsched), i + PF + 1):
                j = wstate["issued"]
                _, src, n = wsched[j]
                P.dma(ring[j % NRING][:, 0:n], src, [], [ring_b[j % NRING]], ring_s[j % NRING], queue="pool")
                wstate["issued"] += 1
            return ring[i % NRING], ring_b[i % NRING]

        def phase0():
            for l in range(L):
                ps, psb = PS[0], PS_b[0]
                for cb in range(6):
                    si = (l * 6 + cb) % 2
                    stg, stg_b = stage[si], stage_b[si]
                    P.dma(stg[:, 0:4096].rearrange("p (k c) -> p k c", k=8),
                          adaw_d[l, :, :, cb * 512:(cb + 1) * 512], [], [stg_b], stage_s[si])

                    def mm(cb=cb):
                        ins = None
                        for nb in range(4):
                            j = cb * 4 + nb
                            for kc in range(8):
                                ins = nc.tensor.matmul(ps[:, j * NSEQ:(j + 1) * NSEQ],
                                                       stg[:, kc * 512 + nb * 128: kc * 512 + nb * 128 + 128],
                                                       cact[:, kc * NSEQ:(kc + 1) * NSEQ],
                                                       start=(kc == 0), stop=(kc == 7))
                        return ins
                    P.op("pe", mm, [stg_b, cact_b], [psb])
                o = l * 24 * NSEQ
                P.op("dve", lambda o=o: nc.vector.tensor_tensor(modT[:, o:o + 24 * NSEQ], ps[:, 0:24 * NSEQ],
                                                                adab[:, o:o + 24 * NSEQ], ALU.add),
                     [psb, misc_b], [mod_b])
                oa = l * 8 * NSEQ
                P.op("dve", lambda o=o, oa=oa: nc.vector.scalar_tensor_tensor(
                    Amod[:, oa:oa + 8 * NSEQ], modT[:, o + 8 * NSEQ:o + 16 * NSEQ], 1.0, ng2[:, oa:oa + 8 * NSEQ],
                    ALU.add, ALU.mult), [mod_b, misc_b], [mod_b])

        def m_shift(l, j, b):
            o = (l * 24 + j) * NSEQ + b
            return modT[:, o:o + 1]

        def m_gate(l, j, b):
            o = (l * 24 + 16 + j) * NSEQ + b
            return modT[:, o:o + 1]

        def m_A(l, j, b):
            o = (l * 8 + j) * NSEQ + b
            return Amod[:, o:o + 1]

        def xsrc(l, b):
            return (xT_d if l == 0 else xs_d)[b].rearrange("(k p) t -> p k t", p=128)

        def xdst(l, b):
            return (outT_d if l == L - 1 else xs_d)[b].rearrange("(k p) t -> p k t", p=128)

        xs_b = [[Buf("xs%d_%d" % (b, t)) for t in range(NTB)] for b in range(NSEQ)]

        def phase1(l, b):
            with contextlib.ExitStack() as ph:
                xt = [sb("p1x%d" % i, [128, 8, 512], F32, ph) for i in range(2)]
                xt_b = [Buf("p1x%d" % i) for i in range(2)]
                xt_s = p1_slots
                sq = [sb("p1sq%d" % i, [128, 512], F32, ph) for i in range(2)]
                sq_b = [Buf("p1sq%d" % i) for i in range(2)]
                tmp = [sb("p1t%d" % i, [128, 512], F32, ph) for i in range(2)]
                tmp_b = [Buf("p1t%d" % i) for i in range(2)]
                srt = sb("p1srt", [128, 512], F32, ph)
                rstd = sb("p1rstd", [128, 512], F32, ph)
                srt_b, rstd_b = Buf("srt"), Buf("rstd")
                src = xsrc(l, b)
                for tb in range(NTB):
                    i = tb % 2
                    P.dma(xt[i][:], src[:, :, tb * 512:(tb + 1) * 512], [xs_b[b][tb]], [xt_b[i]], xt_s[i])
                    ps, psb = PS[tb % 2], PS_b[tb % 2]
                    for j in range(8):
                        k = j % 2
                        P.op("act", lambda j=j, k=k: nc.scalar.activation(out=sq[k][:], in_=xt[i][:, j, :],
                                                                          func=AF.Square), [xt_b[i]], [sq_b[k]])
                        P.op("pe", lambda j=j, k=k: nc.tensor.matmul(ps[:], C("ones"), sq[k][:], start=(j == 0),
                                                                     stop=(j == 7)), [sq_b[k], cst_b], [psb])
                    P.op("act", lambda: nc.scalar.activation(out=srt[:], in_=ps[:], func=AF.Sqrt, bias=1e-6,
                                                             scale=1.0 / D), [psb], [srt_b])
                    P.op("dve", lambda: nc.vector.reciprocal(rstd[:], srt[:]), [srt_b], [rstd_b])
                    for j in range(8):
                        k = j % 2
                        P.op("dve", lambda j=j, k=k: nc.vector.tensor_tensor(tmp[k][:], xt[i][:, j, :], rstd[:],
                                                                             ALU.mult), [xt_b[i], rstd_b], [tmp_b[k]])
                        P.op("act", lambda j=j, k=k: nc.scalar.activation(
                            out=hT[:, j, tb * 512:(tb + 1) * 512], in_=tmp[k][:], func=AF.Identity,
                            bias=m_shift(l, j, b), scale=m_A(l, j, b)), [tmp_b[k], mod_b], [hT_b[tb]])
                P.barrier()

        def phase5(l, b):
            with contextlib.ExitStack() as ph:
                mrg = sb("p5mrg", [128, 8, S], BF16, ph)
                mrg_b = [Buf("mrg%d" % t) for t in range(NTB)]
                g = [sb("p5g%d" % i, [128, 512], F32, ph) for i in range(3)]
                g_b = [Buf("p5g%d" % i) for i in range(3)]
                t_ = [sb("p5t%d" % i, [128, 512], F32, ph) for i in range(3)]
                t_b = [Buf("p5t%d" % i) for i in range(3)]
                xr = [sb("p5x%d" % i, [128, 512], F32, ph) for i in range(2)]
                xr_b = [Buf("p5x%d" % i) for i in range(2)]
                xr_s = p5_slots
                src = xsrc(l, b)
                dst = xdst(l, b)
                for nb in range(8):
                    mgw, mgw_b = load_w(("mg", l, nb))
                    pj, pj_b = load_w(("pj", l, nb))
                    for tb in range(NTB):
                        tok = slice(tb * 512, (tb + 1) * 512)
                        for br in range(3):
                            def mmg(br=br):
                                ins = None
                                for kc in range(8):
                                    ins = nc.tensor.matmul(PS[br][:], mgw[:, kc * 384 + br * 128: kc * 384 + br * 128 + 128],
                                                           hT[:, kc, tok], start=(kc == 0), stop=(kc == 7))
                                return ins
                            P.op("pe", mmg, [mgw_b, hT_b[tb]], [PS_b[br]])
                            P.op("act", lambda br=br: nc.scalar.activation(
                                out=g[br][:], in_=PS[br][:], func=AF.Sigmoid,
                                bias=SPc("mb", l * 24 + br * 8 + nb)), [PS_b[br], spar_b], [g_b[br]])
                        ysrc = [(ysb, ysb_b, 4, 0), (ygdn, ygdn_b, 4, 512), (ygla, ygla_b, 8, 1024)]
                        for br in range(3):
                            yt, yb, nk, wo = ysrc[br]

                            def mmp(yt=yt, nk=nk, wo=wo, br=br):
                                ins = None
                                for kc in range(nk):
                                    ins = nc.tensor.matmul(PS[3 + br][:], pj[:, wo + kc * 128: wo + kc * 128 + 128],
                                                           yt[:, kc, tok], start=(kc == 0), stop=(kc == nk - 1))
                                return ins
                            P.op("pe", mmp, [pj_b] + yb, [PS_b[3 + br]])
                            P.op("dve", lambda br=br: nc.vector.tensor_tensor(t_[br][:], PS[3 + br][:], g[br][:],
                                                                             ALU.mult), [PS_b[3 + br], g_b[br]], [t_b[br]])
                        P.op("pool", lambda: nc.gpsimd.tensor_tensor(t_[0][:], t_[0][:], t_[1][:], ALU.add),
                             [t_b[0], t_b[1]], [t_b[0]])
                        P.op("pool", lambda: nc.gpsimd.tensor_tensor(mrg[:, nb, tok], t_[0][:], t_[2][:], ALU.add),
                             [t_b[0], t_b[2]], [mrg_b[tb]])
                for nb in range(8):
                    wo_t, wo_b = load_w(("wo", l, nb))
                    for tb in range(NTB):
                        tok = slice(tb * 512, (tb + 1) * 512)
                        i = (nb * NTB + tb) % 2
                        ps, psb = PS[6 + i], PS_b[6 + i]
                        P.dma(xr[i][:], src[:, nb, tok], [xs_b[b][tb]], [xr_b[i]], xr_s[i])

                        def mmo():
                            ins = None
                            for kc in range(8):
                                ins = nc.tensor.matmul(ps[:], wo_t[:, kc * 128:(kc + 1) * 128], mrg[:, kc, tok],
                                                       start=(kc == 0), stop=(kc == 7))
                            return ins
                        P.op("pe", mmo, [wo_b, mrg_b[tb]], [psb])
                        P.op("dve", lambda: nc.vector.scalar_tensor_tensor(xr[i][:], ps[:], m_gate(l, nb, b), xr[i][:],
                                                                           ALU.mult, ALU.add), [psb, xr_b[i], mod_b], [xr_b[i]])
                        P.dma(dst[:, nb, tok], xr[i][:], [xr_b[i]], [xo_b[b][tb]], xr_s[i])
                P.barrier()

        xo_b = [[Buf("xo%d_%d" % (b, t)) for t in range(NTB)] for b in range(NSEQ)]

        build_mixers = MIXERS
        ctx = dict(nc=nc, P=P, sb=sb, C=C, SPc=SPc, PS=PS, PS_b=PS_b, hT=hT, hT_b=hT_b, ysb=ysb, ygdn=ygdn,
                   ygla=ygla, ysb_b=ysb_b, ygdn_b=ygdn_b, ygla_b=ygla_b, load_w=load_w, cst_b=cst_b, cb_b=cb_b,
                   spar_b=spar_b, identb=identb, onesb=onesb, nuinclb=nuinclb, sbnegb=sbnegb, sbm01b=sbm01b,
                   ident4b=ident4b, dervp=dervp, derv_b=derv_b, S=S, L=L, NTB=NTB, NT=NT, NSEQ=NSEQ,
                   wsb_d=wsb_d, wgdn_d=wgdn_d, wgla1_d=wgla1_d, wglaz_d=wglaz_d, wba_d=wba_d, wlr_d=wlr_d,
                   w2_d=w2_d, glab_d=glab_d, dbg_d=dbg_d,
                   misc_s=misc_s, spo=spo, spar=spar)

        phase0()
        P.barrier()
        stg_es.close()
        for b in range(NSEQ):
            for l in range(L):
                phase1(l, b)
                for m in build_mixers:
                    m(ctx, l, b)
                phase5(l, b)
                for t in range(NTB):
                    xs_b[b][t].w = xo_b[b][t].w
                    xs_b[b][t].r = {}
                    xo_b[b][t].w = None
        P.barrier()
    return nc, P


def mixer_sb(ctx, l, b):
    g = ctx
    nc, P, sb, C, SPc, PS, PS_b = g["nc"], g["P"], g["sb"], g["C"], g["SPc"], g["PS"], g["PS_b"]
    hT, hT_b, S, NTB, NT = g["hT"], g["hT_b"], g["S"], g["NTB"], g["NT"]
    ysb, ysb_b = g["ysb"], g["ysb_b"]
    cst_b, cb_b, spar_b, derv_b = g["cst_b"], g["cb_b"], g["spar_b"], g["derv_b"]
    with contextlib.ExitStack() as ph:
        def T(name, shape, dt=F32):
            return sb("s" + name, shape, dt, ph), Buf("s" + name)
        qT, q_b = T("q", [128, S], BF16)
        kT, k_b = T("k", [128, S], BF16)
        zg, z_b = T("z", [128, S], BF16)
        v, v_b = T("v", [128, NT, 128], BF16)
        ST = []
        for si in range(2):
            ST.append(dict(
                esp=[T("esp%d_%d" % (si, i), [128, 512]) for i in range(2)],
                sph=[T("sph%d_%d" % (si, i), [128, 512], BF16) for i in range(2)],
                spl=[T("spl%d_%d" % (si, i), [128, 512], BF16) for i in range(2)],
                arg=T("arg%d" % si, [128, 512]),
                W=[T("W%d_%d" % (si, i), [128, 512], BF16) for i in range(2)],
                C=[T("C%d_%d" % (si, i), [128, 512]) for i in range(2)],
                za=[(PS[2 * si], PS_b[2 * si]), (PS[2 * si + 1], PS_b[2 * si + 1])],
                o=(PS[4 + 2 * si], PS_b[4 + 2 * si]), c=(PS[5 + 2 * si], PS_b[5 + 2 * si])))
        tsets = [(ST[si]["esp"][0], ST[si]["esp"][1], ST[si]["arg"]) for si in range(2)]
        for hd in range(4):
            w, w_b = g["load_w"](("sb", l, hd))
            n = 0
            for which in range(2):
                dst, dst_b = (qT, q_b) if which == 0 else (kT, k_b)
                gain = g["dervp"][:, l:l + 1] if which == 0 else SPc("kn", l)
                for tb in range(NTB):
                    tok = slice(tb * 512, (tb + 1) * 512)
                    ps, psb = PS[6 + n % 2], PS_b[6 + n % 2]
                    ps2, ps2b = PS[4 + n % 2], PS_b[4 + n % 2]
                    (sqt, sq_b), (srt, srt_b), (rr, rr_b) = tsets[n % 2]
                    n += 1

                    def mm():
                        ins = None
                        for kc in range(8):
                            ins = nc.tensor.matmul(ps[:], w[:, kc * 512 + which * 128: kc * 512 + which * 128 + 128],
                                                   hT[:, kc, tok], start=(kc == 0), stop=(kc == 7))
                        return ins
                    P.op("pe", mm, [w_b, hT_b[tb]], [psb])
                    P.op("act", lambda: nc.scalar.activation(out=sqt[:], in_=ps[:], func=AF.Square), [psb], [sq_b])
                    P.op("pe", lambda: nc.tensor.matmul(ps2[:], C("ones"), sqt[:], start=True, stop=True),
                         [sq_b, cst_b], [ps2b])
                    P.op("act", lambda: nc.scalar.activation(out=srt[:], in_=ps2[:], func=AF.Sqrt, bias=1e-6,
                                                             scale=1.0 / 128), [ps2b], [srt_b])
                    P.op("dve", lambda: nc.vector.reciprocal(rr[:], srt[:]), [srt_b], [rr_b])
                    P.op("dve", lambda: nc.vector.scalar_tensor_tensor(
                        dst[:, tok], ps[:], gain, rr[:], ALU.mult, ALU.mult), [psb, rr_b, derv_b, spar_b], [dst_b])
            for tb in range(NTB):
                tok = slice(tb * 512, (tb + 1) * 512)
                ps, psb = PS[6 + tb % 2], PS_b[6 + tb % 2]

                def mmz():
                    ins = None
                    for kc in range(8):
                        ins = nc.tensor.matmul(ps[:], w[:, kc * 512 + 384: kc * 512 + 512], hT[:, kc, tok],
                                               start=(kc == 0), stop=(kc == 7))
                    return ins
                P.op("pe", mmz, [w_b, hT_b[tb]], [psb])
                P.op("act", lambda: nc.scalar.activation(out=zg[:, tok], in_=ps[:], func=AF.Silu), [psb], [z_b])
            for t4 in range(NT // 4):
                ps, psb = PS[t4 % 2], PS_b[t4 % 2]

                def mmv():
                    ins = None
                    for tt in range(4):
                        t = t4 * 4 + tt
                        for kc in range(8):
                            ins = nc.tensor.matmul(ps[:, tt * 128:(tt + 1) * 128], hT[:, kc, t * 128:(t + 1) * 128],
                                                   w[:, kc * 512 + 256: kc * 512 + 384], start=(kc == 0), stop=(kc == 7))
                    return ins
                P.op("pe", mmv, [w_b, hT_b[t4]], [psb])
                P.op("act", lambda: nc.scalar.copy(v[:, t4 * 4:(t4 + 1) * 4, :], ps[:].rearrange("p (a c) -> p a c", a=4)),
                     [psb], [v_b])
            qts = [[], []]
            order = list(reversed(range(NTB)))
            for k_, qt in enumerate(order):
                qts[0 if (k_ % 4) in (0, 3) else 1].append(qt)
            tiles = [[(qt, kb) for qt in qts[si] for kb in reversed(range(4 * qt + 4))] for si in range(2)]

            def front(si, i):
                st = ST[si]
                qt, kb = tiles[si][i]
                m = kb - 4 * qt
                qtok = slice(qt * 512, (qt + 1) * 512)
                ktok = slice(kb * 128, (kb + 1) * 128)
                (za, za_b) = st["za"][i % 2]
                (esp, esp_b) = st["esp"][i % 2]
                (sph, sph_b) = st["sph"][i % 2]
                (spl, spl_b) = st["spl"][i % 2]
                P.op("pe", lambda: nc.tensor.matmul(za[:], kT[:, ktok], qT[:, qtok], start=True, stop=True), [k_b, q_b], [za_b])
                P.op("act", lambda: nc.scalar.activation(out=esp[:], in_=za[:], func=AF.Exp), [za_b], [esp_b])
                P.op("act", lambda: nc.scalar.activation(out=esp[:], in_=esp[:], func=AF.Ln, bias=1.0), [esp_b], [esp_b])
                if m >= 0:
                    P.op("pool", lambda: nc.gpsimd.tensor_tensor(esp[:], esp[:], g["sbm01b"][:, m * 512:(m + 1) * 512], ALU.mult),
                         [esp_b, cb_b], [esp_b])
                P.op("dve", lambda: nc.vector.tensor_copy(sph[:], esp[:]), [esp_b], [sph_b])
                P.op("pool", lambda: nc.gpsimd.tensor_tensor(spl[:], esp[:], sph[:], ALU.subtract), [esp_b, sph_b], [spl_b])

            def back(si, i):
                st = ST[si]
                qt, kb = tiles[si][i]
                m = kb - 4 * qt
                first = (kb == 4 * qt + 3)
                qtok = slice(qt * 512, (qt + 1) * 512)
                ktok = slice(kb * 128, (kb + 1) * 128)
                (za, za_b) = st["za"][i % 2]
                (sph, sph_b) = st["sph"][i % 2]
                (spl, spl_b) = st["spl"][i % 2]
                (arg, arg_b) = st["arg"]
                (W, W_b) = st["W"][i % 2]
                (Cc, Cc_b) = st["C"][i % 2]
                (Cp, Cp_b) = st["C"][(i - 1) % 2]
                (ops_, ops_b) = st["o"]
                (cps, cps_b) = st["c"]

                def mma():
                    nc.tensor.matmul(za[:], g["nuinclb"][:], sph[:], start=False, stop=False)
                    ins = nc.tensor.matmul(za[:], g["nuinclb"][:], spl[:], start=False, stop=(m < 0))
                    if m >= 0:
                        ins = nc.tensor.matmul(za[:], g["identb"][:], g["sbnegb"][:, m * 512:(m + 1) * 512], start=False, stop=True)
                    return ins
                P.op("pe", mma, [k_b, q_b, sph_b, spl_b, cb_b], [za_b])
                if kb > 0:
                    def mmc():
                        nc.tensor.matmul(cps[:], g["onesb"][:], sph[:], start=first, stop=False)
                        return nc.tensor.matmul(cps[:], g["onesb"][:], spl[:], start=False, stop=True)
                    P.op("pe", mmc, [sph_b, spl_b, cb_b], [cps_b])
                if first:
                    P.op("act", lambda: nc.scalar.activation(out=W[:], in_=za[:], func=AF.Exp), [za_b], [W_b])
                else:
                    P.op("dve", lambda: nc.vector.tensor_tensor(arg[:], za[:], Cp[:], ALU.subtract), [za_b, Cp_b], [arg_b])
                    P.op("act", lambda: nc.scalar.activation(out=W[:], in_=arg[:], func=AF.Exp), [arg_b], [W_b])
                if kb > 0:
                    P.op("dve", lambda: nc.vector.tensor_copy(Cc[:], cps[:]), [cps_b], [Cc_b])
                P.op("pe", lambda: nc.tensor.matmul(ops_[:], v[:, kb, :], W[:], start=first, stop=(kb == 0)), [v_b, W_b], [ops_b])
                if kb == 0:
                    P.op("dve", lambda: nc.vector.tensor_tensor(ysb[:, hd, qtok], ops_[:], zg[:, qtok], ALU.mult),
                         [ops_b, z_b], [ysb_b[hd]])

            nmax = max(len(t_) for t_ in tiles)
            for si in range(2):
                if len(tiles[si]) > 0:
                    front(si, 0)
            for i in range(nmax):
                for si in range(2):
                    if i + 1 < len(tiles[si]):
                        front(si, i + 1)
                for si in range(2):
                    if i < len(tiles[si]):
                        back(si, i)
        P.barrier()


def mixer_gla(ctx, l, b):
    g = ctx
    nc, P, sb, C, SPc, PS, PS_b = g["nc"], g["P"], g["sb"], g["C"], g["SPc"], g["PS"], g["PS_b"]
    hT, hT_b, S, NTB, NT = g["hT"], g["hT_b"], g["S"], g["NTB"], g["NT"]
    ygla, ygla_b = g["ygla"], g["ygla_b"]
    cst_b, cb_b, spar_b = g["cst_b"], g["cb_b"], g["spar_b"]
    identb, onesb = g["identb"], g["onesb"]
    with contextlib.ExitStack() as ph:
        lrT = sb("glrT", [16, S], BF16, ph)
        lr_b = Buf("lrT")
        w2 = sb("gw2", [16, 512], BF16, ph)
        glab = sb("gglab", [1, 512], BF16, ph)
        w2f = sb("gw2f", [16, 512], F32, ph)
        glabf = sb("gglabf", [1, 512], F32, ph)
        w2_b = Buf("w2")
        qg = sb("gqg", [128, 512], BF16, ph)
        kg = sb("gkg", [128, 512], BF16, ph)
        kb16 = sb("gkb", [128, 512], BF16, ph)
        qk_b = Buf("gqk")
        la = sb("gla", [128, 512], F32, ph)
        la_b = Buf("la")
        ex = sb("gex", [128, 512], F32, ph)
        ex_b = Buf("gex")
        eg = sb("geg", [128, 512], F32, ph)
        eng = sb("geng", [128, 512], F32, ph)
        eg_b = Buf("eg")
        edk = sb("gedk", [128, 512], F32, ph)
        edk_b = Buf("edk")
        kd = sb("gkd", [128, 4, 128], BF16, ph)
        kd_b = Buf("kd")
        att = sb("gatt", [128, 4, 128], BF16, ph)
        att_b = Buf("att")
        v = sb("gv", [128, 4, 256], BF16, ph)
        v_b = Buf("gv")
        zg = sb("gzg", [128, 2, 512], BF16, ph)
        zg_b = Buf("gzg")
        St = sb("gS", [128, 256], F32, ph)
        Sb = sb("gSb", [128, 256], BF16, ph)
        S_b, Sb_b = Buf("gS"), Buf("gSb")
        sq = sb("gsq", [128, 512], F32, ph)
        srt = sb("gsrt", [128, 512], F32, ph)
        rr = sb("grr", [128, 512], F32, ph)
        tmp = sb("gtmp", [128, 512], F32, ph)
        sq_b, srt_b, rr_b, tmp_b = Buf("gsq"), Buf("gsrt"), Buf("grr"), Buf("gtmp")
        P.dma(w2f[:], g["w2_d"][l], [], [w2_b], g["misc_s"])
        P.dma(glabf[:], g["glab_d"][l], [], [w2_b], g["misc_s"])
        P.op("dve", lambda: nc.vector.tensor_copy(w2[:], w2f[:]), [w2_b], [w2_b])
        P.op("dve", lambda: nc.vector.tensor_copy(glab[:], glabf[:]), [w2_b], [w2_b])
        wl, wl_b = g["load_w"](("lr", l))
        for tb in range(NTB):
            tok = slice(tb * 512, (tb + 1) * 512)

            def mml():
                ins = None
                for kc in range(8):
                    ins = nc.tensor.matmul(PS[2][0:16, :], wl[:, kc * 16:(kc + 1) * 16], hT[:, kc, tok],
                                           start=(kc == 0), stop=(kc == 7))
                return ins
            P.op("pe", mml, [wl_b, hT_b[tb]], [PS_b[2]])
            P.op("act", lambda: nc.scalar.copy(lrT[:, tok], PS[2][0:16, :]), [PS_b[2]], [lr_b])
        for hd in range(4):
            w, w_b = g["load_w"](("gla1", l, hd))
            wz, wz_b = g["load_w"](("glaz", l, hd))
            P.op("pool", lambda: nc.gpsimd.memset(St[:], 0.0), [], [S_b])
            P.op("pool", lambda: nc.gpsimd.memset(Sb[:], 0.0), [], [Sb_b])
            for tb in range(NTB):
                tok = slice(tb * 512, (tb + 1) * 512)
                for which in range(2):
                    def mm():
                        ins = None
                        for kc in range(8):
                            ins = nc.tensor.matmul(PS[5 + which][:], w[:, kc * 512 + which * 128: kc * 512 + which * 128 + 128],
                                                   hT[:, kc, tok], start=(kc == 0), stop=(kc == 7))
                        return ins
                    P.op("pe", mm, [w_b, hT_b[tb]], [PS_b[5 + which]])
                def mmx():
                    ins = None
                    for t in range(4):
                        tt = slice(tb * 512 + t * 128, tb * 512 + (t + 1) * 128)
                        nc.tensor.matmul(PS[2][:, t * 128:(t + 1) * 128], lrT[:, tt], w2[:, hd * 128:(hd + 1) * 128],
                                         start=True, stop=False)
                        ins = nc.tensor.matmul(PS[2][:, t * 128:(t + 1) * 128], onesb[0:1, :],
                                               glab[0:1, hd * 128:(hd + 1) * 128], start=False, stop=True)
                    return ins
                P.op("pe", mmx, [lr_b, w2_b, cb_b], [PS_b[2]])
                P.op("act", lambda: nc.scalar.activation(out=ex[:], in_=PS[2][:], func=AF.Exp, scale=-1.0),
                     [PS_b[2]], [ex_b])
                P.op("act", lambda: nc.scalar.activation(out=ex[:], in_=ex[:], func=AF.Ln, bias=1.0), [ex_b], [ex_b])
                P.op("dve", lambda: nc.vector.tensor_scalar(la[:], ex[:], -1.0 / 16.0, None, ALU.mult), [ex_b], [la_b])

                def mmg():
                    ins = None
                    for t in range(4):
                        ins = nc.tensor.matmul(PS[3][:, t * 128:(t + 1) * 128], la[:, t * 128:(t + 1) * 128],
                                               C("utincl"), start=True, stop=True)
                    return ins
                P.op("pe", mmg, [la_b, cst_b], [PS_b[3]])

                def mmt():
                    ins = None
                    for t in range(4):
                        ins = nc.tensor.matmul(PS[2][:, t * 128:(t + 1) * 128], C("ltstrict"),
                                               la[:, t * 128:(t + 1) * 128], start=True, stop=True)
                    return ins
                P.op("pe", mmt, [la_b, cst_b], [PS_b[2]])
                P.op("act", lambda: nc.scalar.activation(out=eg[:], in_=PS[3][:], func=AF.Exp), [PS_b[3]], [eg_b])
                P.op("act", lambda: nc.scalar.activation(out=eng[:], in_=PS[3][:], func=AF.Exp, scale=-1.0),
                     [PS_b[3]], [eg_b])
                P.op("act", lambda: nc.scalar.activation(out=edk[:], in_=PS[2][:], func=AF.Exp), [PS_b[2]], [edk_b])
                P.op("dve", lambda: nc.vector.scalar_tensor_tensor(qg[:], PS[5][:], 128.0 ** -0.5, eg[:], ALU.mult, ALU.mult),
                     [PS_b[5], eg_b], [qk_b])
                P.op("dve", lambda: nc.vector.tensor_tensor(kg[:], PS[6][:], eng[:], ALU.mult), [PS_b[6], eg_b], [qk_b])
                P.op("act", lambda: nc.scalar.copy(kb16[:], PS[6][:]), [PS_b[6]], [qk_b])
                p4 = PS[4][:].bitcast(BF16)

                def mmtr():
                    ins = None
                    for t in range(4):
                        ins = nc.tensor.transpose(p4[:, t * 128:(t + 1) * 128], kb16[:, t * 128:(t + 1) * 128], identb[:])
                    return ins
                P.op("pe", mmtr, [qk_b, cb_b], [PS_b[4]])
                P.op("dve", lambda: nc.vector.tensor_tensor(kd[:].rearrange("p a c -> p (a c)"), p4[:, 0:512], edk[:], ALU.mult),
                     [PS_b[4], edk_b], [kd_b])

                def mma():
                    ins = None
                    for t in range(4):
                        ins = nc.tensor.matmul(PS[5][:, t * 128:(t + 1) * 128], kg[:, t * 128:(t + 1) * 128],
                                               qg[:, t * 128:(t + 1) * 128], start=True, stop=True)
                    return ins
                P.op("pe", mma, [qk_b], [PS_b[5]])
                P.op("dve", lambda: nc.vector.tensor_tensor(att[:].rearrange("p a c -> p (a c)"), PS[5][:], C("m01T_incl4"),
                                                            ALU.mult), [PS_b[5], cst_b], [att_b])
                for half in range(2):
                    def mmv():
                        ins = None
                        for tt in range(2):
                            t = half * 2 + tt
                            ts_ = slice(tb * 512 + t * 128, tb * 512 + (t + 1) * 128)
                            for kc in range(8):
                                ins = nc.tensor.matmul(PS[6][:, tt * 256:(tt + 1) * 256], hT[:, kc, ts_],
                                                       w[:, kc * 512 + 256: kc * 512 + 512], start=(kc == 0), stop=(kc == 7))
                        return ins
                    P.op("pe", mmv, [w_b, hT_b[tb]], [PS_b[6]])
                    P.op("act", lambda: nc.scalar.copy(v[:, half * 2:half * 2 + 2, :].rearrange("p a c -> p (a c)"), PS[6][:]),
                         [PS_b[6]], [v_b])
                for vb in range(2):
                    def mmz():
                        ins = None
                        for kc in range(8):
                            ins = nc.tensor.matmul(PS[6][:], wz[:, kc * 256 + vb * 128: kc * 256 + vb * 128 + 128],
                                                   hT[:, kc, tok], start=(kc == 0), stop=(kc == 7))
                        return ins
                    P.op("pe", mmz, [wz_b, hT_b[tb]], [PS_b[6]])
                    P.op("act", lambda: nc.scalar.activation(out=zg[:, vb, :], in_=PS[6][:], func=AF.Silu), [PS_b[6]], [zg_b])
                for t in range(4):
                    for c in range(2):
                        rng = slice(c * 64, (c + 1) * 64)
                        col = slice(t * 128 + c * 64, t * 128 + (c + 1) * 64)

                        def mmo():
                            ins = None
                            for vb in range(2):
                                nc.tensor.matmul(PS[vb][:, col], Sb[:, vb * 128:(vb + 1) * 128], qg[:, col],
                                                 start=True, stop=False)
                                ins = nc.tensor.matmul(PS[vb][:, col], v[rng, t, vb * 128:(vb + 1) * 128],
                                                       att[rng, t, c * 64:(c + 1) * 64], start=False, stop=True)
                            return ins
                        P.op("pe", mmo, [Sb_b, qk_b, v_b, att_b], [PS_b[0], PS_b[1]])
                        P.op("pe", lambda: nc.tensor.matmul(PS[7][:, 0:256], kd[rng, t, :], v[rng, t, :], start=True, stop=True),
                             [kd_b, v_b], [PS_b[7]])
                        last = t * 128 + c * 64 + 63
                        P.op("dve", lambda: nc.vector.scalar_tensor_tensor(St[:], St[:], eg[:, last:last + 1], PS[7][:, 0:256],
                                                                           ALU.mult, ALU.add), [S_b, eg_b, PS_b[7]], [S_b])
                        P.op("act", lambda: nc.scalar.copy(Sb[:], St[:]), [S_b], [Sb_b])
                for vb in range(2):
                    P.op("act", lambda: nc.scalar.activation(out=sq[:], in_=PS[vb][:], func=AF.Square), [PS_b[vb]], [sq_b])
                    P.op("pe", lambda: nc.tensor.matmul(PS[3][:], C("ones"), sq[:], start=(vb == 0), stop=(vb == 1)),
                         [sq_b, cst_b], [PS_b[3]])
                P.op("act", lambda: nc.scalar.activation(out=srt[:], in_=PS[3][:], func=AF.Sqrt, bias=1e-6, scale=1.0 / 256),
                     [PS_b[3]], [srt_b])
                P.op("dve", lambda: nc.vector.reciprocal(rr[:], srt[:]), [srt_b], [rr_b])
                for vb in range(2):
                    P.op("dve", lambda: nc.vector.scalar_tensor_tensor(tmp[:], PS[vb][:], SPc("lon", l * 2 + vb), rr[:],
                                                                       ALU.mult, ALU.mult), [PS_b[vb], rr_b, spar_b], [tmp_b])
                    P.op("pool", lambda: nc.gpsimd.tensor_tensor(ygla[:, hd * 2 + vb, tok], tmp[:], zg[:, vb, :], ALU.mult),
                         [tmp_b, zg_b], [ygla_b[hd]])
        P.barrier()


GSTOP = [0]


class _Stop(Exception):
    pass


def _stage(k):
    if GSTOP[0] == k:
        raise _Stop()


def mixer_gdn(ctx, l, b):
    with contextlib.ExitStack() as ph:
        try:
            _mixer_gdn(ctx, l, b, ph)
        except _Stop:
            ctx["P"].barrier()


def _mixer_gdn(ctx, l, b, ph):
    g = ctx
    nc, P, sb, C, SPc, PS, PS_b = g["nc"], g["P"], g["sb"], g["C"], g["SPc"], g["PS"], g["PS_b"]
    hT, hT_b, S, NTB, NT, L = g["hT"], g["hT_b"], g["S"], g["NTB"], g["NT"], g["L"]
    ygdn, ygdn_b = g["ygdn"], g["ygdn_b"]
    cst_b, cb_b, spar_b, derv_b = g["cst_b"], g["cb_b"], g["spar_b"], g["derv_b"]
    identb, ident4b, dervp = g["identb"], g["ident4b"], g["dervp"]
    NCH = S // 64
    if True:
        def T(name, shape, dt=F32):
            return sb("d" + name, shape, dt, ph), Buf("d" + name)
        w_ba, wba_b = g["load_w"](("ba", l))
        e_t, e_tb = T("e_t", [128, NT * 4])
        lnb_t, lnb_tb = T("lnb_t", [128, NT * 4])
        g_t, g_tb = T("g_t", [128, NT * 4])
        gc_t, gc_tb = T("gc_t", [128, NT * 4])
        g_hm, g_hmb = T("g_hm", [128, 4 * NT])
        lnb_hm, lnb_hmb = T("lnb_hm", [128, 4 * NT])
        beta_hm, beta_hmb = T("beta_hm", [128, 4 * NT])
        bek_hm, bek_hmb = T("bek_hm", [128, 4 * NT])
        e_c, e_cb = T("e_c", [64, NCH * 4])
        lnb_c, lnb_cb = T("lnb_c", [64, NCH * 4])
        g_c, g_cb = T("g_c", [64, NCH * 4])
        x_c, x_cb = T("x_c", [64, NCH * 4])
        g_chm, g_chmb = T("g_chm", [64, 4 * NCH])
        kdec_chm, kdec_chmb = T("kdec_chm", [64, 4 * NCH])
        dtb = SPc("dtb", l * 4, 4)
        nexpA = dervp[:, L + l * 4: L + l * 4 + 4]

        def gates(np_, n, ps, psb, e, eb, lnb, lnbb, gg, ggb, tokfn):
            def mm():
                ins = None
                for i in range(n):
                    for kc in range(8):
                        ins = nc.tensor.matmul(ps[0:np_, i * 8:(i + 1) * 8], hT[:, kc, tokfn(i)], w_ba[:, kc * 8:(kc + 1) * 8],
                                               start=(kc == 0), stop=(kc == 7))
                return ins
            P.op("pe", mm, [wba_b] + hT_b, [psb])
            pv = ps[0:np_, 0:n * 8].rearrange("p (t e) -> p t e", e=8)
            e3 = e[:].rearrange("p (t h) -> p t h", h=4)
            l3 = lnb[:].rearrange("p (t h) -> p t h", h=4)
            g3 = gg[:].rearrange("p (t h) -> p t h", h=4)
            P.op("act", lambda: nc.scalar.activation(out=e3, in_=pv[:, :, 0:4], func=AF.Exp, scale=-1.0), [psb], [eb])
            P.op("act", lambda: nc.scalar.activation(out=lnb[:], in_=e[:], func=AF.Ln, bias=1.0), [eb], [lnbb])
            P.op("dve", lambda: nc.vector.tensor_scalar(lnb[:], lnb[:], -1.0, None, ALU.mult), [lnbb], [lnbb])
            P.op("dve", lambda: nc.vector.tensor_tensor(e3, pv[:, :, 4:8], dtb[0:np_, :].unsqueeze(1).to_broadcast([np_, n, 4]),
                                                        ALU.add), [psb, spar_b, lnbb], [eb])
            P.op("act", lambda: nc.scalar.activation(out=e[:], in_=e[:], func=AF.Exp), [eb], [eb])
            P.op("act", lambda: nc.scalar.activation(out=e[:], in_=e[:], func=AF.Ln, bias=1.0), [eb], [eb])
            P.op("dve", lambda: nc.vector.tensor_tensor(g3, e3, nexpA[0:np_, :].unsqueeze(1).to_broadcast([np_, n, 4]), ALU.mult),
                 [eb, derv_b], [ggb])

        gates(128, NT, PS[0], PS_b[0], e_t, e_tb, lnb_t, lnb_tb, g_t, g_tb, lambda i: slice(i * 128, (i + 1) * 128))
        gates(64, NCH, PS[1], PS_b[1], e_c, e_cb, lnb_c, lnb_cb, g_c, g_cb, lambda i: slice(i * 64, (i + 1) * 64))

        def hm(dst, src, n):
            return dst.rearrange("p (h t) -> p t h", h=4), src.rearrange("p (t h) -> p t h", h=4)
        def mmgc():
            ins = None
            for t in range(NT):
                ins = nc.tensor.matmul(PS[0][:, t * 4:(t + 1) * 4], C("utincl"), g_t[:, t * 4:(t + 1) * 4], start=True, stop=True)
            return ins
        P.op("pe", mmgc, [g_tb, cst_b], [PS_b[0]])
        P.op("dve", lambda: nc.vector.tensor_tensor(gc_t[:], PS[0][:, 0:NT * 4], lnb_t[:], ALU.add), [PS_b[0], lnb_tb], [gc_tb])
        d_, s_ = hm(bek_hm[:], gc_t[:], NT)
        P.op("act", lambda: nc.scalar.activation(out=d_, in_=s_, func=AF.Exp), [gc_tb], [bek_hmb])
        d_, s_ = hm(beta_hm[:], lnb_t[:], NT)
        P.op("act", lambda: nc.scalar.activation(out=d_, in_=s_, func=AF.Exp), [lnb_tb], [beta_hmb])
        d_, s_ = hm(g_hm[:], g_t[:], NT)
        P.op("dve", lambda: nc.vector.tensor_copy(d_, s_), [g_tb], [g_hmb])
        d_, s_ = hm(lnb_hm[:], lnb_t[:], NT)
        P.op("dve", lambda: nc.vector.tensor_copy(d_, s_), [lnb_tb], [lnb_hmb])
        def mmtc():
            ins = None
            for c in range(NCH):
                ins = nc.tensor.matmul(PS[1][0:64, c * 4:(c + 1) * 4], C("ltstrict")[0:64, 0:64], g_c[:, c * 4:(c + 1) * 4],
                                       start=True, stop=True)
            return ins
        P.op("pe", mmtc, [g_cb, cst_b], [PS_b[1]])
        d_ = kdec_chm[:].rearrange("p (h t) -> p t h", h=4)
        P.op("act", lambda: nc.scalar.activation(out=d_, in_=PS[1][0:64, 0:NCH * 4].rearrange("p (t h) -> p t h", h=4),
                                                 func=AF.Exp), [PS_b[1]], [kdec_chmb])
        d_, s_ = hm(g_chm[:], g_c[:], NCH)
        P.op("dve", lambda: nc.vector.tensor_copy(d_, s_), [g_cb], [g_chmb])

        _stage(1)
        xc = [T("xc%d" % i, [128, 515]) for i in range(3)]
        acc, acc_b = T("acc", [128, 512])
        sil, sil_b = T("sil", [128, 512])
        sq, sq_b = T("sq", [128, 512])
        srt, srt_b = T("srt", [128, 512])
        rr, rr_b = T("rr", [128, 512])
        qT, q_b = T("qT", [128, 512], BF16)
        kT, k_b = T("kT", [128, 512], BF16)
        vT, v_b = T("vT", [128, 512], BF16)
        zg, zg_b = T("zg", [128, 512], BF16)
        Gb, Gb_b = T("Gb", [128, 512])
        Gbc, Gbc_b = T("Gbc", [64, 512])
        Dm, Dm_b = T("Dm", [128, 512])
        Ebs, Ebs_b = T("Ebs", [128, 512])
        ET, ET_b = T("ET", [64, 512])
        egr, egr_b = T("egr", [128, 512])
        qd, qd_b = T("qd", [128, 512], BF16)
        Ap = [T("Ap%d" % i, [128, 512], BF16) for i in range(2)]
        Bp = [T("Bp%d" % i, [128, 512], BF16) for i in range(2)]
        TT, TT_b = T("TT", [128, 512], BF16)
        Kbg, Kbg_b = T("Kbg", [128, 4, 128], BF16)
        Vb, Vb_b = T("Vb", [128, 4, 128], BF16)
        Kdc, Kdc_b = T("Kdc", [64, 8, 128], BF16)
        qkT, qkT_b = T("qkT", [64, 512], BF16)
        ub, ub_b = T("ub", [64, 8, 128])
        wT, wT_b = T("wT", [128, 512], BF16)
        ubf, ubf_b = T("ubf", [64, 128], BF16)
        St, S_b = T("S", [128, 128])
        Sb, Sb_b = T("Sb", [128, 128], BF16)
        tmp, tmp_b = T("tmp", [128, 512])
        p4 = PS[4][:].bitcast(BF16)
        for hd in range(4):
            w, w_b = g["load_w"](("gdn", l, hd))
            P.op("pool", lambda: nc.gpsimd.memset(St[:], 0.0), [], [S_b])
            P.op("pool", lambda: nc.gpsimd.memset(Sb[:], 0.0), [], [Sb_b])
            for i in range(3):
                P.op("pool", lambda: nc.gpsimd.memset(xc[i][0][:, 0:3], 0.0), [], [xc[i][1]])
            for tb in range(NTB):
                tok = slice(tb * 512, (tb + 1) * 512)
                acc_s = [(acc, acc_b), (Gb, Gb_b), (Dm, Dm_b)]
                sil_s = [(sil, sil_b), (Ebs, Ebs_b)]
                sq_s = [(sq, sq_b), (tmp, tmp_b)]
                srt_s = [(srt, srt_b), (rr, rr_b)]
                pq = [0, 2, 6]
                for which in range(3):
                    def mm():
                        ins = None
                        for kc in range(8):
                            ins = nc.tensor.matmul(PS[pq[which]][:], w[:, kc * 512 + which * 128: kc * 512 + which * 128 + 128],
                                                   hT[:, kc, tok], start=(kc == 0), stop=(kc == 7))
                        return ins
                    P.op("pe", mm, [w_b, hT_b[tb]], [PS_b[pq[which]]])
                for which in range(3):
                    xcw, xcw_b = xc[which]
                    if tb > 0:
                        P.op("pool", lambda: nc.gpsimd.tensor_copy(xcw[:, 0:3], xcw[:, 512:515]), [xcw_b], [xcw_b])
                    P.op("act", lambda: nc.scalar.copy(xcw[:, 3:515], PS[pq[which]][:]), [PS_b[pq[which]]], [xcw_b])
                cw = lambda which, tap: SPc("conv", l * 48 + (which * 4 + hd) * 4 + tap)
                for which in range(3):
                    xcw, xcw_b = xc[which]
                    a_, a_b = acc_s[which]
                    P.op("act", lambda: nc.scalar.activation(out=a_[:], in_=xcw[:, 0:512], func=AF.Identity, scale=cw(which, 0)),
                         [xcw_b, spar_b], [a_b])
                for tap in range(1, 4):
                    for which in range(3):
                        xcw, xcw_b = xc[which]
                        a_, a_b = acc_s[which]
                        P.op("dve", lambda: nc.vector.scalar_tensor_tensor(a_[:], xcw[:, tap:tap + 512], cw(which, tap), a_[:],
                                                                           ALU.mult, ALU.add), [xcw_b, spar_b, a_b], [a_b])
                for which in range(3):
                    a_, a_b = acc_s[which]
                    if which == 2:
                        P.op("act", lambda: nc.scalar.activation(out=vT[:], in_=a_[:], func=AF.Silu), [a_b], [v_b])
                    else:
                        s_, s_b = sil_s[which]
                        P.op("act", lambda: nc.scalar.activation(out=s_[:], in_=a_[:], func=AF.Silu), [a_b], [s_b])
                for which in range(2):
                    s_, s_b = sil_s[which]
                    q_, q_b2 = sq_s[which]
                    P.op("act", lambda: nc.scalar.activation(out=q_[:], in_=s_[:], func=AF.Square), [s_b], [q_b2])
                stp = [3, 5]
                for which in range(2):
                    q_, q_b2 = sq_s[which]
                    P.op("pe", lambda: nc.tensor.matmul(PS[stp[which]][:], C("ones"), q_[:], start=True, stop=True),
                         [q_b2, cst_b], [PS_b[stp[which]]])
                for which in range(2):
                    r_, r_b = srt_s[which]
                    P.op("act", lambda: nc.scalar.activation(out=r_[:], in_=PS[stp[which]][:], func=AF.Sqrt, bias=1e-6),
                         [PS_b[stp[which]]], [r_b])
                for which in range(2):
                    r_, r_b = srt_s[which]
                    P.op("dve", lambda: nc.vector.reciprocal(r_[:], r_[:]), [r_b], [r_b])
                for which in range(2):
                    s_, s_b = sil_s[which]
                    r_, r_b = srt_s[which]
                    dst, dst_b = (qT, q_b) if which == 0 else (kT, k_b)
                    sc = 128.0 ** -0.5 if which == 0 else 1.0
                    P.op("dve", lambda: nc.vector.scalar_tensor_tensor(dst[:], s_[:], sc, r_[:], ALU.mult, ALU.mult),
                         [s_b, r_b], [dst_b])
                _stage(2)
                def mmz():
                    ins = None
                    for kc in range(8):
                        ins = nc.tensor.matmul(PS[0][:], w[:, kc * 512 + 384: kc * 512 + 512], hT[:, kc, tok],
                                               start=(kc == 0), stop=(kc == 7))
                    return ins
                P.op("pe", mmz, [w_b, hT_b[tb]], [PS_b[0]])
                P.op("act", lambda: nc.scalar.activation(out=zg[:], in_=PS[0][:], func=AF.Silu), [PS_b[0]], [zg_b])
                gsl = g_hm[:, hd * NT + tb * 4: hd * NT + tb * 4 + 4]
                P.op("dve", lambda: nc.vector.tensor_copy(Gb[:].rearrange("p (t c) -> p t c", c=128),
                                                          gsl.unsqueeze(2).to_broadcast([128, 4, 128])), [g_hmb], [Gb_b])
                gcs = g_chm[:, hd * NCH + tb * 8: hd * NCH + tb * 8 + 8]
                P.op("dve", lambda: nc.vector.tensor_copy(Gbc[:].rearrange("p (t c) -> p t c", c=64),
                                                          gcs.unsqueeze(2).to_broadcast([64, 8, 64])), [g_chmb], [Gbc_b])

                def mmD():
                    ins = None
                    for t in range(4):
                        cs = slice(t * 128, (t + 1) * 128)
                        nc.tensor.matmul(PS[2][:, cs], C("utincl"), Gb[:, cs], start=True, stop=False)
                        ins = nc.tensor.matmul(PS[2][:, cs], Gb[:, cs], C("nutincl"), start=False, stop=True)
                    return ins
                P.op("pe", mmD, [Gb_b, cst_b], [PS_b[2]])
                P.op("dve", lambda: nc.vector.tensor_tensor(Dm[:], PS[2][:], C("negm_strict4"), ALU.add), [PS_b[2], cst_b], [Dm_b])
                for t in range(4):
                    cs = slice(t * 128, (t + 1) * 128)
                    bi = hd * NT + tb * 4 + t
                    P.op("act", lambda: nc.scalar.activation(out=Ebs[:, cs], in_=Dm[:, cs], func=AF.Exp, bias=lnb_hm[:, bi:bi + 1]),
                         [Dm_b, lnb_hmb], [Ebs_b])
                _stage(3)
                def mmG():
                    ins = None
                    for t in range(4):
                        cs = slice(t * 128, (t + 1) * 128)
                        ins = nc.tensor.matmul(PS[3][:, cs], Gb[:, cs], C("utincl"), start=True, stop=True)
                    return ins
                P.op("pe", mmG, [Gb_b, cst_b], [PS_b[3]])
                P.op("act", lambda: nc.scalar.activation(out=egr[:], in_=PS[3][:], func=AF.Exp), [PS_b[3]], [egr_b])
                P.op("pool", lambda: nc.gpsimd.tensor_tensor(qd[:], qT[:], egr[:], ALU.mult), [q_b, egr_b], [qd_b])
                _stage(4)
                def mmM():
                    ins = None
                    for t in range(4):
                        cs = slice(t * 128, (t + 1) * 128)
                        ins = nc.tensor.matmul(PS[2][:, cs], kT[:, cs], kT[:, cs], start=True, stop=True)
                    return ins
                P.op("pe", mmM, [k_b], [PS_b[2]])
                A0, A0_b = Ap[0]
                P.op("dve", lambda: nc.vector.tensor_tensor(A0[:], PS[2][:], Ebs[:], ALU.mult), [PS_b[2], Ebs_b], [A0_b])

                def mmB():
                    ins = None
                    for t in range(4):
                        cs = slice(t * 128, (t + 1) * 128)
                        ins = nc.tensor.transpose(p4[:, cs], A0[:, cs], identb[:])
                    return ins
                P.op("pe", mmB, [A0_b, cb_b], [PS_b[4]])
                B0, B0_b = Bp[0]
                P.op("dve", lambda: nc.vector.scalar_tensor_tensor(TT[:], p4[:, 0:512], -1.0, ident4b[:], ALU.mult, ALU.add),
                     [PS_b[4], cb_b], [TT_b])
                P.op("dve", lambda: nc.vector.tensor_copy(B0[:], p4[:, 0:512]), [PS_b[4]], [B0_b])
                _stage(5)
                cur = 0
                for lev in range(5):
                    Ac, Ac_b = Ap[cur]
                    Bc, Bc_b = Bp[cur]
                    An, An_b = Ap[1 - cur]
                    Bn, Bn_b = Bp[1 - cur]

                    def mmA2():
                        ins = None
                        for t in range(4):
                            cs = slice(t * 128, (t + 1) * 128)
                            ins = nc.tensor.matmul(PS[2][:, cs], Bc[:, cs], Ac[:, cs], start=True, stop=True)
                        return ins
                    P.op("pe", mmA2, [Ac_b, Bc_b], [PS_b[2]])
                    P.op("act", lambda: nc.scalar.copy(An[:], PS[2][:]), [PS_b[2]], [An_b])
                    if lev < 4:
                        def mmB2():
                            ins = None
                            for t in range(4):
                                cs = slice(t * 128, (t + 1) * 128)
                                ins = nc.tensor.matmul(PS[3][:, cs], Ac[:, cs], Bc[:, cs], start=True, stop=True)
                            return ins
                        P.op("pe", mmB2, [Ac_b, Bc_b], [PS_b[3]])
                        P.op("dve", lambda: nc.vector.tensor_copy(Bn[:], PS[3][:]), [PS_b[3]], [Bn_b])

                    def mmT():
                        ins = None
                        for t in range(4):
                            cs = slice(t * 128, (t + 1) * 128)
                            ins = nc.tensor.matmul(PS[5][:, cs], An[:, cs], TT[:, cs], start=True, stop=True)
                        return ins
                    P.op("pe", mmT, [An_b, TT_b], [PS_b[5]])
                    P.op("dve", lambda: nc.vector.tensor_tensor(TT[:], TT[:], PS[5][:], ALU.add), [PS_b[5], TT_b], [TT_b])
                    cur = 1 - cur
                _stage(6)
                def mmKt():
                    ins = None
                    for t in range(4):
                        cs = slice(t * 128, (t + 1) * 128)
                        ins = nc.tensor.transpose(p4[:, cs], kT[:, cs], identb[:])
                    return ins
                P.op("pe", mmKt, [k_b, cb_b], [PS_b[4]])
                ci = hd * NT + tb * 4
                P.op("dve", lambda: nc.vector.tensor_tensor(Kbg[:], p4[:, 0:512].rearrange("p (t c) -> p t c", c=128),
                                                            bek_hm[:, ci:ci + 4].unsqueeze(2).to_broadcast([128, 4, 128]), ALU.mult),
                     [PS_b[4], bek_hmb], [Kbg_b])

                def mmVt():
                    ins = None
                    for t in range(4):
                        cs = slice(t * 128, (t + 1) * 128)
                        ins = nc.tensor.transpose(p4[:, cs], vT[:, cs], identb[:])
                    return ins
                P.op("pe", mmVt, [v_b, cb_b], [PS_b[4]])
                P.op("dve", lambda: nc.vector.tensor_tensor(Vb[:], p4[:, 0:512].rearrange("p (t c) -> p t c", c=128),
                                                            beta_hm[:, ci:ci + 4].unsqueeze(2).to_broadcast([128, 4, 128]), ALU.mult),
                     [PS_b[4], beta_hmb], [Vb_b])
                _stage(7)
                def mmKc():
                    ins = None
                    for ch in range(8):
                        ins = nc.tensor.transpose(p4[0:64, ch * 128:(ch + 1) * 128], kT[:, ch * 64:(ch + 1) * 64], identb[:])
                    return ins
                P.op("pe", mmKc, [k_b, cb_b], [PS_b[4]])
                cj = hd * NCH + tb * 8
                P.op("dve", lambda: nc.vector.tensor_tensor(Kdc[:], p4[0:64, :].rearrange("p (t c) -> p t c", c=128),
                                                            kdec_chm[:, cj:cj + 8].unsqueeze(2).to_broadcast([64, 8, 128]), ALU.mult),
                     [PS_b[4], kdec_chmb], [Kdc_b])
                _stage(8)
                def mmDc():
                    ins = None
                    for ch in range(8):
                        cs = slice(ch * 64, (ch + 1) * 64)
                        nc.tensor.matmul(PS[5][0:64, cs], C("utincl")[0:64, 0:64], Gbc[:, cs], start=True, stop=False)
                        ins = nc.tensor.matmul(PS[5][0:64, cs], Gbc[:, cs], C("nutincl")[0:64, 0:64], start=False, stop=True)
                    return ins
                P.op("pe", mmDc, [Gbc_b, cst_b], [PS_b[5]])
                negT = C("negmT_incl4")[0:64, 0:64].unsqueeze(1).to_broadcast([64, 8, 64])
                P.op("dve", lambda: nc.vector.tensor_tensor(ET[:].rearrange("p (t c) -> p t c", c=64), negT,
                                                            PS[5][0:64, :].rearrange("p (t c) -> p t c", c=64), ALU.subtract),
                     [PS_b[5], cst_b], [ET_b])
                P.op("act", lambda: nc.scalar.activation(out=ET[:], in_=ET[:], func=AF.Exp), [ET_b], [ET_b])

                def mmQK():
                    ins = None
                    for ch in range(8):
                        cs = slice(ch * 64, (ch + 1) * 64)
                        ins = nc.tensor.matmul(PS[3][0:64, cs], kT[:, cs], qT[:, cs], start=True, stop=True)
                    return ins
                P.op("pe", mmQK, [k_b, q_b], [PS_b[3]])
                P.op("dve", lambda: nc.vector.tensor_tensor(qkT[:], PS[3][0:64, :], ET[:], ALU.mult), [PS_b[3], ET_b], [qkT_b])
                _stage(9)
                for half in range(2):
                    def mmU():
                        ins = None
                        for k4 in range(4):
                            ch = half * 4 + k4
                            t, c = ch // 2, ch % 2
                            ins = nc.tensor.matmul(PS[6 + half][0:64, k4 * 128:(k4 + 1) * 128],
                                                   TT[:, t * 128 + c * 64: t * 128 + (c + 1) * 64], Vb[:, t, :], start=True, stop=True)
                        return ins
                    P.op("pe", mmU, [TT_b, Vb_b], [PS_b[6 + half]])
                    P.op("act", lambda: nc.scalar.copy(ub[:, half * 4:(half + 1) * 4, :].rearrange("p a c -> p (a c)"),
                                                       PS[6 + half][0:64, :]), [PS_b[6 + half]], [ub_b])

                def mmW():
                    ins = None
                    for ch in range(8):
                        t, c = ch // 2, ch % 2
                        ins = nc.tensor.matmul(PS[5][:, ch * 64:(ch + 1) * 64], Kbg[:, t, :],
                                               TT[:, t * 128 + c * 64: t * 128 + (c + 1) * 64], start=True, stop=True)
                    return ins
                P.op("pe", mmW, [Kbg_b, TT_b], [PS_b[5]])
                P.op("dve", lambda: nc.vector.tensor_copy(wT[:], PS[5][:]), [PS_b[5]], [wT_b])
                _stage(10)
                for ch in range(8):
                    cs = slice(ch * 64, (ch + 1) * 64)
                    P.op("pe", lambda: nc.tensor.matmul(PS[6][0:64, 0:128], wT[:, cs], Sb[:], start=True, stop=True),
                         [wT_b, Sb_b], [PS_b[6]])
                    P.op("dve", lambda: nc.vector.tensor_tensor(ubf[:], ub[:, ch, :], PS[6][0:64, 0:128], ALU.subtract),
                         [ub_b, PS_b[6]], [ubf_b])

                    def mmO():
                        nc.tensor.matmul(PS[1][:, cs], Sb[:], qd[:, cs], start=True, stop=False)
                        return nc.tensor.matmul(PS[1][:, cs], ubf[:], qkT[:, cs], start=False, stop=True)
                    P.op("pe", mmO, [Sb_b, qd_b, ubf_b, qkT_b], [PS_b[1]])
                    P.op("pe", lambda: nc.tensor.matmul(PS[7][:, 0:128], Kdc[:, ch, :], ubf[:], start=True, stop=True),
                         [Kdc_b, ubf_b], [PS_b[7]])
                    last = ch * 64 + 63
                    P.op("dve", lambda: nc.vector.scalar_tensor_tensor(St[:], St[:], egr[:, last:last + 1], PS[7][:, 0:128],
                                                                       ALU.mult, ALU.add), [S_b, egr_b, PS_b[7]], [S_b])
                    P.op("act", lambda: nc.scalar.copy(Sb[:], St[:]), [S_b], [Sb_b])
                _stage(11)
                P.op("act", lambda: nc.scalar.activation(out=sq[:], in_=PS[1][:], func=AF.Square), [PS_b[1]], [sq_b])
                P.op("pe", lambda: nc.tensor.matmul(PS[7][:], C("ones"), sq[:], start=True, stop=True), [sq_b, cst_b], [PS_b[7]])
                P.op("act", lambda: nc.scalar.activation(out=srt[:], in_=PS[7][:], func=AF.Sqrt, bias=1e-6, scale=1.0 / 128),
                     [PS_b[7]], [srt_b])
                P.op("dve", lambda: nc.vector.reciprocal(rr[:], srt[:]), [srt_b], [rr_b])
                P.op("dve", lambda: nc.vector.scalar_tensor_tensor(tmp[:], PS[1][:], SPc("gon", l), rr[:], ALU.mult, ALU.mult),
                     [PS_b[1], rr_b, spar_b], [tmp_b])
                P.op("pool", lambda: nc.gpsimd.tensor_tensor(ygdn[:, hd, tok], tmp[:], zg[:], ALU.mult), [tmp_b, zg_b], [ygdn_b[hd]])
        P.barrier()


def mixer_fake(which):
    def f(ctx, l, b):
        g = ctx
        nc, P = g["nc"], g["P"]
        hT, hT_b = g["hT"], g["hT_b"]
        if which == "sb":
            P.op("pool", lambda: nc.gpsimd.tensor_copy(g["ysb"][:], hT[:, 0:4, :]), hT_b, g["ysb_b"])
        elif which == "gdn":
            P.op("pool", lambda: nc.gpsimd.tensor_copy(g["ygdn"][:], hT[:, 4:8, :]), hT_b, g["ygdn_b"])
        else:
            P.op("pool", lambda: nc.gpsimd.tensor_copy(g["ygla"][:], hT[:, :, :]), hT_b, g["ygla_b"])
    return f


MIXERS = [mixer_sb, mixer_gdn, mixer_gla]


def kernel(**inputs):
    NSEQ, S, L, NCORE = 2, 2048, 4, 8
    inp = {k: np.asarray(v) for k, v in inputs.items()}
    common = prep_common(inp, L, NSEQ)
    nc, _ = build_program(NSEQ, S, L)
    x = inp["x"]
    c = inp["c"]
    in_maps = []
    for core in range(NCORE):
        m = dict(common)
        xb = x[core * NSEQ:(core + 1) * NSEQ]
        m["xT"] = np.ascontiguousarray(xb.transpose(0, 2, 1))
        cb = c[core * NSEQ:(core + 1) * NSEQ]
        m["cT"] = np.ascontiguousarray(cb.reshape(NSEQ, 8, 128).transpose(2, 1, 0).reshape(128, 8 * NSEQ))
        in_maps.append(m)
    res = run_bass_kernel_spmd(nc, in_maps, core_ids=list(range(NCORE)))
    outs = [np.asarray(r["outT"]).transpose(0, 2, 1) for r in res.results]
    return np.ascontiguousarray(np.concatenate(outs, axis=0).astype(np.float32))
```
